# Optimizing a Trainium2 kernel written in Bass

```python
import math
import jax, jax.numpy as jnp
from jax import lax
import numpy as np

D_MODEL = 4096
BATCH = 4
SEQ = 2048
DEPTH = 2

GRID_W = 64
CTX_LEN = 256
N_MIXERS = 2
N_HY = (DEPTH + 1) // 2
N_NA = DEPTH // 2
HY_ORDER = 2
HY_EMB = 33
HY_BANDS = (HY_EMB - 1) // 2
HY_FILT = 64
HY_SHORT = 3
HY_FAST_DECAY = 0.3
HY_SLOW_DECAY = 1.5
HY_TARGET = 1e-2
HY_SHIFT = 0.05
NA_HEAD_DIM = 128
NA_HEADS = D_MODEL // NA_HEAD_DIM
WIN_H = 8
WIN_W = 16
PEER_HEADS = 8
PEER_KEYS = 128
PEER_EXPERTS = PEER_KEYS * PEER_KEYS
PEER_DKEY = 256
PEER_TOPK = 16
PEER_BLOCK = 128
EPS = 1e-6

kernel_name = 'hybrid_hyena_natten_peer_dit'


def rmsnorm(x, g):
    xf = x.astype(jnp.float32)
    y = xf * lax.rsqrt(jnp.mean(xf * xf, axis=-1, keepdims=True) + EPS)
    return (y * g.astype(jnp.float32)).astype(x.dtype)


def modulate(h, shift, scale):
    return h * (1 + scale) + shift


def ada_mod(cvec, w, b):
    m = jax.nn.silu(cvec) @ w + b
    return jnp.split(m[..., None, :], 6, axis=-1)


def short_conv(u, w, b):
    L = u.shape[1]
    p = HY_SHORT // 2
    up = jnp.pad(u, ((0, 0), (p, HY_SHORT - 1 - p), (0, 0)))
    out = b
    for tap in range(HY_SHORT):
        out = out + up[:, tap:tap + L] * w[tap]
    return out


def hyena_filters(L, w1, b1, w2, b2, w3, b3, w4, freq):
    f32 = jnp.float32
    t = jnp.linspace(0.0, 1.0, L, dtype=f32)[:, None]
    wpos = (2.0 * math.pi / L) * jnp.arange(L, dtype=f32)[:, None]
    bands = jnp.linspace(1e-4, HY_BANDS - 1, HY_BANDS, dtype=f32)[None, :]
    z = jnp.concatenate([t, jnp.cos(bands * wpos), -jnp.sin(bands * wpos)], axis=-1)
    fr = freq.astype(f32)
    h = jnp.sin(fr * (z @ w1.astype(f32) + b1.astype(f32)))
    h = jnp.sin(fr * (h @ w2.astype(f32) + b2.astype(f32)))
    h = jnp.sin(fr * (h @ w3.astype(f32) + b3.astype(f32)))
    h = (h @ w4.astype(f32)).reshape(L, 2, HY_ORDER, D_MODEL)
    max_decay = math.log(HY_TARGET) / HY_FAST_DECAY
    min_decay = math.log(HY_TARGET) / HY_SLOW_DECAY
    deltas = jnp.abs(jnp.linspace(min_decay, max_decay, D_MODEL, dtype=f32))
    window = jnp.exp(-t * deltas) + HY_SHIFT
    h = h * window[:, None, None, :]
    filt = jnp.concatenate([h[:, 0], jnp.zeros((1, HY_ORDER, D_MODEL), f32), h[:0:-1, 1]], axis=0)
    filt = filt / jnp.sum(jnp.abs(filt), axis=0, keepdims=True)
    return jnp.fft.rfft(filt, axis=0)


def long_conv(u, filt_f, skip):
    L = u.shape[1]
    uf = jnp.fft.rfft(u.astype(jnp.float32), n=2 * L, axis=1)
    y = jnp.fft.irfft(uf * filt_f[None], n=2 * L, axis=1)[:, :L]
    return (y + u.astype(jnp.float32) * skip.astype(jnp.float32)).astype(u.dtype)


def hyena_mix(h, w_in, b_in, conv_w, conv_b, filt_params, skip, w_out, b_out):
    L = h.shape[1]
    proj = short_conv(h @ w_in + b_in, conv_w, conv_b)
    v, x1, x2 = jnp.split(proj, 3, axis=-1)
    filt_f = hyena_filters(L, *filt_params)
    z = x1 * long_conv(v, filt_f[:, 0], skip[0])
    z = x2 * long_conv(z, filt_f[:, 1], skip[1])
    return z @ w_out + b_out


def na_mix(hx, hc, w_qkv, q_g, k_g, rpb, w_out, with_ctx_out):
    B, L, _ = hx.shape
    rows = L // GRID_W
    kh = min(WIN_H, rows)
    scale = NA_HEAD_DIM ** -0.5
    w_q, w_k, w_v = jnp.split(w_qkv, 3, axis=1)

    def split_heads(t):
        return t.reshape(t.shape[:2] + (NA_HEADS, NA_HEAD_DIM))

    def queries(h):
        return rmsnorm(split_heads(h @ w_q), q_g) * scale

    def keys(h):
        return rmsnorm(split_heads(h @ w_k), k_g)

    def values(h):
        return split_heads(h @ w_v)

    kc, vc = keys(hc), values(hc)
    grid = (B, rows, GRID_W, NA_HEADS, NA_HEAD_DIM)
    qg = queries(hx).reshape(grid)
    kg = keys(hx).reshape(grid)
    vg = values(hx).reshape(grid)
    cols = jnp.arange(GRID_W)
    c_start = jnp.clip(cols - WIN_W // 2, 0, GRID_W - WIN_W)
    col_in = (cols[None, :] >= c_start[:, None]) & (cols[None, :] < c_start[:, None] + WIN_W)
    col_idx = jnp.clip(cols[None, :] - cols[:, None], -(WIN_W - 1), WIN_W - 1) + (WIN_W - 1)
    rpb_cols = rpb.astype(jnp.float32)[:, :, col_idx]
    n_loc = kh * GRID_W

    def row_step(r):
        r0 = jnp.clip(r - kh // 2, 0, rows - kh)
        q_r = lax.dynamic_index_in_dim(qg, r, axis=1, keepdims=False)
        k_b = lax.dynamic_slice_in_dim(kg, r0, kh, axis=1)
        v_b = lax.dynamic_slice_in_dim(vg, r0, kh, axis=1)
        row_idx = r0 + jnp.arange(kh) - r + (WIN_H - 1)
        bias = jnp.transpose(rpb_cols[:, row_idx], (0, 2, 1, 3))
        s_loc = jnp.einsum('bqhd,brkhd->bhqrk', q_r, k_b, preferred_element_type=jnp.float32) + bias[None]
        s_loc = jnp.where(col_in[None, None, :, None, :], s_loc, -jnp.inf)
        s_ctx = jnp.einsum('bqhd,bchd->bhqc', q_r, kc, preferred_element_type=jnp.float32)
        s = jnp.concatenate([s_loc.reshape(B, NA_HEADS, GRID_W, n_loc), s_ctx], axis=-1)
        p = jax.nn.softmax(s, axis=-1).astype(vg.dtype)
        p_loc = p[..., :n_loc].reshape(B, NA_HEADS, GRID_W, kh, GRID_W)
        return (jnp.einsum('bhqrk,brkhd->bqhd', p_loc, v_b)
                + jnp.einsum('bhqc,bchd->bqhd', p[..., n_loc:], vc))

    o = lax.map(row_step, jnp.arange(rows))
    yx = jnp.moveaxis(o, 0, 1).reshape(B, L, D_MODEL) @ w_out
    yc = None
    if with_ctx_out:
        qc = queries(hc)
        s = jnp.einsum('bqhd,bkhd->bhqk', qc, kc, preferred_element_type=jnp.float32)
        p = jax.nn.softmax(s, axis=-1).astype(vc.dtype)
        yc = jnp.einsum('bhqk,bkhd->bqhd', p, vc).reshape(hc.shape) @ w_out
    return yx, yc


def peer(h, w_q, k1, k2, u_tab, v_tab):
    B, L, D = h.shape
    half = PEER_DKEY // 2
    n_cand = PEER_TOPK * PEER_TOPK

    def block(xb):
        q = (xb @ w_q).reshape(PEER_BLOCK, PEER_HEADS, 2, half)
        s1 = jnp.einsum('thd,hnd->thn', q[:, :, 0], k1, preferred_element_type=jnp.float32)
        s2 = jnp.einsum('thd,hnd->thn', q[:, :, 1], k2, preferred_element_type=jnp.float32)
        v1, i1 = lax.top_k(s1, PEER_TOPK)
        v2, i2 = lax.top_k(s2, PEER_TOPK)
        cand = (v1[..., :, None] + v2[..., None, :]).reshape(PEER_BLOCK, PEER_HEADS, n_cand)
        cand_id = (i1[..., :, None] * PEER_KEYS + i2[..., None, :]).reshape(PEER_BLOCK, PEER_HEADS, n_cand)
        score, pos = lax.top_k(cand, PEER_TOPK)
        eid = jnp.take_along_axis(cand_id, pos, axis=-1)
        g = jax.nn.softmax(score, axis=-1)
        act = jax.nn.gelu(jnp.einsum('td,thkd->thk', xb, u_tab[eid], preferred_element_type=jnp.float32), approximate=False)
        return jnp.einsum('thk,thkd->td', (g * act).astype(v_tab.dtype), v_tab[eid])

    out = lax.map(block, h.reshape(B * L // PEER_BLOCK, PEER_BLOCK, D))
    return out.reshape(B, L, D)


def setup_inputs(seed: int = 0) -> dict:
    key = jax.random.key(seed)
    ks = iter(jax.random.split(key, 40))

    def nrm(shape, scale):
        return jax.random.normal(next(ks), shape, jnp.float32) * scale

    D = D_MODEL
    dinv = D ** -0.5
    return {
        'x': nrm((BATCH, SEQ, D), 1.0),
        'c': nrm((BATCH, D), 1.0),
        'ctx': nrm((BATCH, CTX_LEN, D), 1.0),
        'c_ctx': nrm((D,), 1.0),
        'ada_w': nrm((DEPTH, D, 6 * D), 0.5 * dinv),
        'ada_b': nrm((DEPTH, 6 * D), 0.01),
        'norm1_g': 1.0 + nrm((DEPTH, D), 0.01),
        'norm2_g': 1.0 + nrm((DEPTH, D), 0.01),
        'hy_w_in': nrm((N_HY, D, 3 * D), dinv),
        'hy_b_in': nrm((N_HY, 3 * D), 0.01),
        'hy_conv_w': nrm((N_HY, HY_SHORT, 3 * D), 0.5),
        'hy_conv_b': nrm((N_HY, 3 * D), 0.01),
        'hy_f_w1': nrm((N_HY, HY_EMB, HY_FILT), HY_EMB ** -0.5),
        'hy_f_b1': nrm((N_HY, HY_FILT), 0.1),
        'hy_f_w2': nrm((N_HY, HY_FILT, HY_FILT), HY_FILT ** -0.5),
        'hy_f_b2': nrm((N_HY, HY_FILT), 0.1),
        'hy_f_w3': nrm((N_HY, HY_FILT, HY_FILT), HY_FILT ** -0.5),
        'hy_f_b3': nrm((N_HY, HY_FILT), 0.1),
        'hy_f_w4': nrm((N_HY, HY_FILT, 2 * HY_ORDER * D), HY_FILT ** -0.5),
        'hy_f_freq': 1.0 + nrm((N_HY, HY_FILT), 0.01),
        'hy_skip': nrm((N_HY, HY_ORDER, D), 1.0),
        'hy_w_out': nrm((N_HY, D, D), dinv),
        'hy_b_out': nrm((N_HY, D), 0.01),
        'na_w_qkv': nrm((N_NA, D, 3 * D), dinv),
        'na_q_g': 1.0 + nrm((N_NA, NA_HEAD_DIM), 0.01),
        'na_k_g': 1.0 + nrm((N_NA, NA_HEAD_DIM), 0.01),
        'na_rpb': nrm((N_NA, NA_HEADS, 2 * WIN_H - 1, 2 * WIN_W - 1), 0.1),
        'na_w_out': nrm((N_NA, D, D), dinv),
        'peer_wq': nrm((DEPTH, D, PEER_HEADS * PEER_DKEY), dinv),
        'peer_k1': nrm((DEPTH, PEER_HEADS, PEER_KEYS, PEER_DKEY // 2), (PEER_DKEY // 2) ** -0.5),
        'peer_k2': nrm((DEPTH, PEER_HEADS, PEER_KEYS, PEER_DKEY // 2), (PEER_DKEY // 2) ** -0.5),
        'peer_u': nrm((DEPTH, PEER_EXPERTS, D), dinv),
        'peer_v': nrm((DEPTH, PEER_EXPERTS, D), 0.5),
    }


def reference(x, c, ctx, c_ctx, ada_w, ada_b, norm1_g, norm2_g,
              hy_w_in, hy_b_in, hy_conv_w, hy_conv_b, hy_f_w1, hy_f_b1, hy_f_w2, hy_f_b2,
              hy_f_w3, hy_f_b3, hy_f_w4, hy_f_freq, hy_skip, hy_w_out, hy_b_out,
              na_w_qkv, na_q_g, na_k_g, na_rpb, na_w_out,
              peer_wq, peer_k1, peer_k2, peer_u, peer_v):
    hx, hc = x, ctx
    for i in range(DEPTH):
        last = i == DEPTH - 1
        j = i // N_MIXERS
        mx = ada_mod(c, ada_w[i], ada_b[i])
        mc = ada_mod(c_ctx, ada_w[i], ada_b[i])
        hx_n = modulate(rmsnorm(hx, norm1_g[i]), mx[0], mx[1])
        if i % N_MIXERS == 0:
            filt_params = (hy_f_w1[j], hy_f_b1[j], hy_f_w2[j], hy_f_b2[j],
                           hy_f_w3[j], hy_f_b3[j], hy_f_w4[j], hy_f_freq[j])
            hy_args = (hy_w_in[j], hy_b_in[j], hy_conv_w[j], hy_conv_b[j], filt_params,
                       hy_skip[j], hy_w_out[j], hy_b_out[j])
            yx = hyena_mix(hx_n, *hy_args)
            yc = None
            if not last:
                yc = hyena_mix(modulate(rmsnorm(hc, norm1_g[i]), mc[0], mc[1]), *hy_args)
        else:
            hc_n = modulate(rmsnorm(hc, norm1_g[i]), mc[0], mc[1])
            yx, yc = na_mix(hx_n, hc_n, na_w_qkv[j], na_q_g[j], na_k_g[j], na_rpb[j], na_w_out[j],
                            not last)
        peer_args = (peer_wq[i], peer_k1[i], peer_k2[i], peer_u[i], peer_v[i])
        hx = hx + mx[2] * yx
        hx = hx + mx[5] * peer(modulate(rmsnorm(hx, norm2_g[i]), mx[3], mx[4]), *peer_args)
        if not last:
            hc = hc + mc[2] * yc
            hc = hc + mc[5] * peer(modulate(rmsnorm(hc, norm2_g[i]), mc[3], mc[4]), *peer_args)
    return hx
```

```python
import math
from contextlib import ExitStack
import numpy as np
import ml_dtypes
import concourse.bass as bass
import concourse.mybir as mybir
from concourse.bass_utils import run_bass_kernel_spmd

F32 = mybir.dt.float32
BF16 = mybir.dt.bfloat16
AF = mybir.ActivationFunctionType
ALU = mybir.AluOpType
AX = mybir.AxisListType
NCORES = 8

CFG_FULL = dict(D=4096, B=4, L=2048, CTX=256, GW=64, WIN_H=8, WIN_W=16, HD=128,
                PH=8, PK=128, PDK=256, TOPK=16, EMB=33, FILT=64, DEPTH=2)


class Buf:
    def __init__(self, h, name):
        self.h = h
        self.name = name
        self.lw = None
        self.rd = {}

    def __getitem__(self, idx):
        return self.h[idx]

    def ap(self):
        return self.h.ap() if hasattr(self.h, "ap") else self.h[:]


class Prog:
    NDMA = {"sp": 16, "pool": 2, "act": 2}

    def __init__(self, nc, es):
        self.nc = nc
        self.es = es
        self.engs = {"pe": nc.tensor, "act": nc.scalar, "dve": nc.vector, "pool": nc.gpsimd, "sp": nc.sync}
        self.sems = {}
        self.cnt = {}
        for e in ("pe", "act", "dve", "pool"):
            self.sems[e] = es.enter_context(nc.semaphore("s_" + e))
            self.cnt[e] = 0
        self.dma_i = {}
        for q in ("sp", "pool", "act"):
            self.dma_i[q] = 0
            for i in range(self.NDMA[q]):
                k = ("dma", q, i)
                self.sems[k] = es.enter_context(nc.semaphore("s_dma_%s_%d" % (q, i)))
                self.cnt[k] = 0
        self.sems["cc"] = es.enter_context(nc.semaphore("s_cc"))
        self.cnt["cc"] = 0
        self.waited = {e: {} for e in self.engs}
        self.nbuf = 0
        self.ninst = 0

    def sb(self, shape, dt, name=None, es=None):
        self.nbuf += 1
        name = (name or "sb") + "_%d" % self.nbuf
        b = Buf((es or self.es).enter_context(self.nc.sbuf_tensor(name, list(shape), dt)), name)
        nbytes = int(np.prod(shape[1:])) * (2 if dt == BF16 else 4)
        rem = nbytes % 64
        if rem:
            (es or self.es).enter_context(self.nc.sbuf_tensor(name + "_pad", [shape[0], 64 - rem], mybir.dt.uint8))
        return b

    def barrier(self):
        for e in self.engs:
            for k, v in self.cnt.items():
                if v > 0:
                    self._wait(e, k, v)

    def ps(self, shape, dt, name=None):
        self.nbuf += 1
        name = (name or "ps") + "_%d" % self.nbuf
        return Buf(self.es.enter_context(self.nc.psum_tensor(name, list(shape), dt)), name)

    def dram(self, shape, dt, name=None, kind=None):
        self.nbuf += 1
        name = name or ("dr_%d" % self.nbuf)
        if kind:
            return Buf(self.nc.dram_tensor(name, list(shape), dt, kind=kind), name)
        return Buf(self.nc.dram_tensor(name, list(shape), dt), name)

    def _wait(self, eng, key, val):
        w = self.waited[eng]
        if w.get(key, 0) >= val:
            return
        w[key] = val
        self.engs[eng].wait_ge(self.sems[key], val)

    def _deps(self, eng, r, w):
        deps = {}

        def add(k, v):
            if deps.get(k, 0) < v:
                deps[k] = v
        for b in r:
            if b.lw:
                add(*b.lw)
        for b in w:
            if b.lw:
                add(*b.lw)
            for k, v in b.rd.items():
                add(k, v)
        for k, v in deps.items():
            if eng == "pe" and k == "pe":
                continue
            self._wait(eng, k, v)

    def _done(self, ev, r, w):
        for b in w:
            b.lw = ev
            b.rd = {}
        for b in r:
            if b in w:
                continue
            if b.rd.get(ev[0], 0) < ev[1]:
                b.rd[ev[0]] = ev[1]

    def op(self, eng, fn, r=(), w=()):
        self._deps(eng, r, w)
        ins = fn(self.engs[eng])
        self.cnt[eng] += 1
        ins.then_inc(self.sems[eng], 1)
        self._done((eng, self.cnt[eng]), r, w)
        self.ninst += 1
        return ins

    def dma(self, out, in_, r=(), w=(), q="sp"):
        self._deps(q, r, w)
        i = self.dma_i[q]
        self.dma_i[q] += 1
        k = ("dma", q, i % self.NDMA[q])
        self._wait(q, k, self.cnt[k])
        ins = self.engs[q].dma_start(out=out, in_=in_)
        self.cnt[k] += 16
        ins.then_inc(self.sems[k], 16)
        self._done((k, self.cnt[k]), r, w)
        self.ninst += 1

    def allgather(self, out_b, in_b):
        self._deps("pool", [in_b], [out_b])
        ins = self.nc.gpsimd.collective_compute("AllGather", ALU.bypass, replica_groups=[list(range(NCORES))],
                                                ins=[in_b.ap()], outs=[out_b.ap()])
        self.cnt["cc"] += 1
        ins.then_inc(self.sems["cc"])
        self._done(("cc", self.cnt["cc"]), [in_b], [out_b])

    def finish(self, bufs):
        for b in bufs:
            if b.lw:
                self._wait("sp", *b.lw)


def _cdiv(a, b):
    return (a + b - 1) // b


class Builder:
    def __init__(self, cfg, debug=()):
        self.cfg = cfg
        self.debug = set(debug)
        self.inputs = {}

    def build(self):
        cfg = self.cfg
        D, L, CTX = cfg["D"], cfg["L"], cfg["CTX"]
        T = L + CTX
        KC = D // 128
        nc = bass.Bass("TRN2", target_bir_lowering=False)
        self.nc = nc
        es = ExitStack()
        self.es = es
        p = Prog(nc, es)
        self.p = p
        self.T, self.KC = T, KC

        def inp(name, shape, dt=F32):
            b = p.dram(shape, dt, name=name, kind="ExternalInput")
            self.inputs[name] = (tuple(shape), dt)
            return b
        self.inp = inp
        self.outs = {}

        def outp(name, shape, dt=F32):
            b = p.dram(shape, dt, name=name, kind="ExternalOutput")
            self.outs[name] = b
            return b
        self.outp = outp

        self.psf = [p.ps([128, 512], F32, "psf") for _ in range(5)]
        self.psx = p.ps([128, 512], F32, "psx")
        self.psb = [p.ps([128, 1024], BF16, "psb") for _ in range(2)]
        self.psf_i = 0
        self.psb_i = 0

        ident_b_d = inp("ident_b", [128, 128], BF16)
        ident_f_d = inp("ident_f", [128, 128], F32)
        self.ident_b = p.sb([128, 128], BF16, "identb")
        self.ident_f = p.sb([128, 128], F32, "identf")
        p.dma(self.ident_b[:], ident_b_d[:, :], r=[ident_b_d], w=[self.ident_b])
        p.dma(self.ident_f[:], ident_f_d[:, :], r=[ident_f_d], w=[self.ident_f])
        self.ones_f = p.sb([128, 128], F32, "onesf")
        p.op("dve", lambda e: e.memset(self.ones_f[:], 1.0), w=[self.ones_f])
        self.eps_t = p.sb([128, 1], F32, "eps")
        p.op("dve", lambda e: e.memset(self.eps_t[:], 1e-6), w=[self.eps_t])

        self.HX = p.dram([T, D], F32, "HX")
        x_in = inp("x", [L, D])
        ctx_in = inp("ctx", [CTX, D])
        p.dma(self.HX[0:L, :], x_in[:, :], r=[x_in], w=[self.HX])
        p.dma(self.HX[L:T, :], ctx_in[:, :], r=[ctx_in], w=[self.HX])

        self.phase_weights()
        self.phase_ada()
        for l in range(cfg["DEPTH"]):
            if "stopw" in self.debug:
                break
            self.layer(l)
            if "only0" in self.debug:
                break

        out = outp("out", [L, D])
        p.dma(out[:, :], self.HX[0:L, :], r=[self.HX], w=[out])
        p.finish(list(self.outs.values()))
        es.close()
        return nc

    def next_psf(self):
        b = self.psf[self.psf_i % len(self.psf)]
        self.psf_i += 1
        return b

    def next_psb(self):
        b = self.psb[self.psb_i % len(self.psb)]
        self.psb_i += 1
        return b

    def gather_weight(self, name, K, N, src_dt=F32):
        p = self.p
        Ks = K // NCORES
        src = self.inp(name, [Ks, N], src_dt)
        full = p.dram([K, N], BF16, name + "_full")
        if src_dt == BF16:
            bounce = p.dram([Ks, N], BF16, name + "_bn")
            p.dma(bounce[:, :], src[:, :], r=[src], w=[bounce])
            p.allgather(full, bounce)
            return full
        bounce = p.dram([Ks, N], BF16, name + "_bn")
        RT = min(128, Ks)
        CT = min(N, 4096)
        engs = ["dve", "pool", "act"]
        for r0 in range(0, Ks, RT):
            for c0 in range(0, N, CT):
                i = self.wi
                self.wi += 1
                st, sb_ = self.wst[i % 2], self.wsb[i % 2]
                p.dma(st[0:RT, 0:CT], src[r0:r0 + RT, c0:c0 + CT], r=[src], w=[st], q="sp")
                e = engs[i % 3]
                if e == "act":
                    p.op("act", lambda E: E.copy(out=sb_[0:RT, 0:CT], in_=st[0:RT, 0:CT]), r=[st], w=[sb_])
                else:
                    p.op(e, lambda E: E.tensor_copy(out=sb_[0:RT, 0:CT], in_=st[0:RT, 0:CT]), r=[st], w=[sb_])
                p.dma(bounce[r0:r0 + RT, c0:c0 + CT], sb_[0:RT, 0:CT], r=[sb_], w=[bounce], q="sp")
        p.allgather(full, bounce)
        return full

    def phase_weights(self):
        cfg, p = self.cfg, self.p
        D, L, CTX = cfg["D"], cfg["L"], cfg["CTX"]
        E = cfg["PK"] ** 2
        NQ = cfg["PH"] * cfg["PDK"]
        self.wi = 0
        with ExitStack() as ph:
            self.wst = [p.sb([128, 4096], F32, "wst", es=ph) for _ in range(2)]
            self.wsb = [p.sb([128, 4096], BF16, "wsb", es=ph) for _ in range(2)]
            W = {}
            W["hy_w_in"] = self.gather_weight("hy_w_in", D, 3 * D)
            W["hy_w_out"] = self.gather_weight("hy_w_out", D, D)
            W["na_w_qkv"] = self.gather_weight("na_w_qkv", D, 3 * D)
            W["na_w_out"] = self.gather_weight("na_w_out", D, D)
            for l in range(cfg["DEPTH"]):
                W["peer_wq%d" % l] = self.gather_weight("peer_wq%d" % l, NQ, D)
                W["peer_uT%d" % l] = self.gather_weight("peer_uT%d" % l, E, D)
                W["peer_v%d" % l] = self.gather_weight("peer_v%d" % l, E, D)
            W["wf_l"] = self.gather_weight("wf_l", 2 * L, L, BF16)
            W["wi_l"] = self.gather_weight("wi_l", L, 2 * L, BF16)
            W["wf_c"] = self.gather_weight("wf_c", 2 * CTX, CTX, BF16)
            W["wi_c"] = self.gather_weight("wi_c", CTX, 2 * CTX, BF16)
            self.W = W
            p.barrier()

    def phase_ada(self):
        cfg, p = self.cfg, self.p
        D, KC, DEPTH = cfg["D"], self.KC, cfg["DEPTH"]
        NL = 6 * D // NCORES
        c_all = self.inp("c_all", [5, D])
        ada_w = self.inp("ada_w", [DEPTH, D, NL])
        ada_b = self.inp("ada_b", [DEPTH, NL])
        self.sel_x_d = self.inp("sel_x", [5, 128])
        self.sel_c_d = self.inp("sel_c", [5, 128])
        self.sel_x = p.sb([5, 128], F32, "selx")
        self.sel_c = p.sb([5, 128], F32, "selc")
        p.dma(self.sel_x[:], self.sel_x_d[:, :], r=[self.sel_x_d], w=[self.sel_x])
        p.dma(self.sel_c[:], self.sel_c_d[:, :], r=[self.sel_c_d], w=[self.sel_c])
        mod_loc = p.dram([DEPTH, 5, NL], F32, "mod_loc")
        self.MOD = p.dram([NCORES, DEPTH, 5, 6, D // NCORES], F32, "MOD")
        NT = _cdiv(NL, 512)
        with ExitStack() as ph:
            cs = p.sb([5, D], F32, "cs", es=ph)
            p.dma(cs[:], c_all[:, :], r=[c_all], w=[cs])
            p.op("act", lambda e: e.activation(out=cs[:], in_=cs[:], func=AF.Silu), r=[cs], w=[cs])
            sT = p.sb([128, KC, 5], F32, "sT", es=ph)
            pst = self.next_psf()
            for k in range(KC):
                p.op("pe", lambda e: e.transpose(out=pst[:, k * 5:(k + 1) * 5], in_=cs[0:5, k * 128:(k + 1) * 128],
                                                 identity=self.ident_f[0:5, 0:5]), r=[cs, self.ident_f], w=[pst])
            p.op("dve", lambda e: e.tensor_copy(out=sT[:].rearrange("p k m -> p (k m)"), in_=pst[:, 0:KC * 5]),
                 r=[pst], w=[sT])
            wt = [p.sb([128, NL], F32, "adaw", es=ph) for _ in range(2)]
            bt = p.sb([5, NL], F32, "adab", es=ph)
            res = p.sb([5, NL], F32, "adares", es=ph)
            for l in range(DEPTH):
                p.dma(bt[:], ada_b[l, :].partition_broadcast(5), r=[ada_b], w=[bt])
                pss = [self.next_psf() for _ in range(min(NT, 5))] + ([self.psx] if NT == 6 else [])
                for k in range(KC):
                    w = wt[k % 2]
                    p.dma(w[:], ada_w[l, k * 128:(k + 1) * 128, :], r=[ada_w], w=[w])
                    for j in range(NT):
                        n0, n1 = j * 512, min(NL, (j + 1) * 512)
                        p.op("pe", lambda e: e.matmul(pss[j][0:5, 0:n1 - n0], lhsT=sT[:, k, :], rhs=w[:, n0:n1],
                                                      start=(k == 0), stop=(k == KC - 1)), r=[sT, w], w=[pss[j]])
                for j in range(NT):
                    n0, n1 = j * 512, min(NL, (j + 1) * 512)
                    p.op("dve", lambda e: e.tensor_tensor(out=res[:, n0:n1], in0=pss[j][0:5, 0:n1 - n0], in1=bt[:, n0:n1],
                                                          op=ALU.add), r=[pss[j], bt], w=[res])
                p.dma(mod_loc[l, :, :], res[:], r=[res], w=[mod_loc])
            p.allgather(self.MOD, mod_loc)
            p.barrier()

    def mod_tile(self, dst, l, k, stream, plus_one=False):
        cfg, p = self.cfg, self.p
        D = cfg["D"]
        rows = self.modrows
        p.dma(rows[:].rearrange("m (r i) -> m r i", r=NCORES),
              self.MOD[:, l, :, k, :].rearrange("r m i -> m r i"), r=[self.MOD], w=[rows])
        sel = self.sel_x if stream == "x" else self.sel_c
        for n0 in range(0, D, 512):
            n1 = min(D, n0 + 512)
            ps = self.next_psf()
            p.op("pe", lambda e: e.matmul(ps[:, 0:n1 - n0], lhsT=sel[:], rhs=rows[:, n0:n1], start=True, stop=True),
                 r=[sel, rows], w=[ps])
            if plus_one:
                p.op("dve", lambda e: e.tensor_scalar(out=dst[:, n0:n1], in0=ps[:, 0:n1 - n0], scalar1=1.0, scalar2=None,
                                                      op0=ALU.add), r=[ps], w=[dst])
            else:
                p.op("dve", lambda e: e.tensor_copy(out=dst[:, n0:n1], in_=ps[:, 0:n1 - n0]), r=[ps], w=[dst])

    def bcast_row(self, dst, src_ap, src_buf, n):
        self.p.dma(dst[:, 0:n], src_ap.partition_broadcast(128), r=[src_buf], w=[dst])

    def phase_norm(self, l, sub, gname, tiles_x, tiles_c, XT):
        cfg, p = self.cfg, self.p
        D, KC = cfg["D"], self.KC
        g_d = self.g_in[gname]
        with ExitStack() as ph:
            self.modrows = p.sb([5, D], F32, "modrows", es=ph)
            gt = p.sb([128, D], F32, "gt", es=ph)
            self.bcast_row(gt, g_d[l, :], g_d, D)
            A1 = p.sb([128, D], F32, "modA", es=ph)
            B1 = p.sb([128, D], F32, "modB", es=ph)
            A = {"x": A1, "c": A1}
            Bt = {"x": B1, "c": B1}
            old = True
            if old:
                A["c"] = p.sb([128, D], F32, "modA", es=ph)
                Bt["c"] = p.sb([128, D], F32, "modB", es=ph)
                for stream, tiles in (("x", tiles_x), ("c", tiles_c)):
                    if tiles:
                        self.mod_tile(A[stream], l, 3 * sub + 1, stream, plus_one=True)
                        p.op("pool", lambda e: e.tensor_tensor(out=A[stream][:], in0=A[stream][:], in1=gt[:], op=ALU.mult),
                             r=[A[stream], gt], w=[A[stream]])
                        self.mod_tile(Bt[stream], l, 3 * sub, stream)
            if "dummy" in self.debug:
                _d1 = p.sb([128, D], F32, "dummy", es=ph)
                _d2 = p.sb([128, D], F32, "dummy", es=ph)
            xt = [p.sb([128, D], F32, "nx", es=ph) for _ in range(2)]
            yb = [p.sb([128, D], BF16, "ny", es=ph) for _ in range(2)]
            junk = p.sb([128, D], BF16, "njunk", es=ph)
            ss = [p.sb([128, 1], F32, "nss", es=ph) for _ in range(2)]
            xT = [p.sb([128, KC, 128], BF16, "nxT", es=ph) for _ in range(2)]
            it = 0
            for stream, tiles in (("x", tiles_x), ("c", tiles_c)):
                if not tiles:
                    continue
                if not old:
                    self.mod_tile(A1, l, 3 * sub + 1, stream, plus_one=True)
                    p.op("pool", lambda e: e.tensor_tensor(out=A1[:], in0=A1[:], in1=gt[:], op=ALU.mult), r=[A1, gt], w=[A1])
                    self.mod_tile(B1, l, 3 * sub, stream)
                for j in tiles:
                    x_, y_, s_, xT_ = xt[it % 2], yb[it % 2], ss[it % 2], xT[it % 2]
                    it += 1
                    p.dma(x_[:], self.HX[j * 128:(j + 1) * 128, :], r=[self.HX], w=[x_])
                    p.op("act", lambda e: e.activation(out=junk[:], in_=x_[:], func=AF.Square, accum_out=s_[:]),
                         r=[x_], w=[junk, s_])
                    p.op("dve", lambda e: e.tensor_scalar(out=s_[:], in0=s_[:], scalar1=1.0 / D, scalar2=1e-6,
                                                          op0=ALU.mult, op1=ALU.add), r=[s_], w=[s_])
                    p.op("act", lambda e: e.activation(out=s_[:], in_=s_[:], func=AF.Sqrt), r=[s_], w=[s_])
                    p.op("dve", lambda e: e.reciprocal(out=s_[:], in_=s_[:]), r=[s_], w=[s_])
                    p.op("dve", lambda e: e.scalar_tensor_tensor(out=x_[:], in0=x_[:], scalar=s_[:, 0:1], in1=A[stream][:],
                                                                 op0=ALU.mult, op1=ALU.mult), r=[x_, s_, A[stream]], w=[x_])
                    p.op("dve" if "nopool" in self.debug else "pool", lambda e: e.tensor_tensor(out=y_[:], in0=x_[:], in1=Bt[stream][:], op=ALU.add),
                         r=[x_, Bt[stream]], w=[y_])
                    for k0 in range(0, KC, 8):
                        pb = self.next_psb()
                        kn = min(8, KC - k0)
                        for k in range(k0, k0 + kn):
                            p.op("pe", lambda e: e.transpose(out=pb[:, (k - k0) * 128:(k - k0 + 1) * 128],
                                                             in_=y_[:, k * 128:(k + 1) * 128], identity=self.ident_b[:]),
                                 r=[y_, self.ident_b], w=[pb])
                        eng = "act" if (k0 // 8) % 2 else "dve"
                        if eng == "act":
                            p.op("act", lambda e: e.copy(out=xT_[:, k0:k0 + kn, :].rearrange("p k t -> p (k t)"),
                                                         in_=pb[:, 0:kn * 128]), r=[pb], w=[xT_])
                        else:
                            p.op("dve", lambda e: e.tensor_copy(out=xT_[:, k0:k0 + kn, :].rearrange("p k t -> p (k t)"),
                                                                in_=pb[:, 0:kn * 128]), r=[pb], w=[xT_])
                    p.dma(XT[j, :, :, :], xT_[:], r=[xT_], w=[XT])
            p.barrier()

    def layer(self, l):
        cfg, p = self.cfg, self.p
        D, L, CTX, T, KC = cfg["D"], cfg["L"], cfg["CTX"], self.T, self.KC
        last = l == cfg["DEPTH"] - 1
        if l == 0:
            self.g_in = {"norm1_g": self.inp("norm1_g", [cfg["DEPTH"], D]), "norm2_g": self.inp("norm2_g", [cfg["DEPTH"], D])}
            self.XT = p.dram([T // 128, 128, KC, 128], BF16, "XT")
        tiles_x = list(range(L // 128))
        tiles_c = list(range(L // 128, T // 128))
        self.phase_norm(l, 0, "norm1_g", tiles_x, [] if "noc" in self.debug else tiles_c, self.XT)
        if "xt1_%d" % l in self.debug:
            o = self.outp("dbg_xt1_%d" % l, [T // 128, 128, KC, 128], BF16)
            p.dma(o.ap(), self.XT.ap(), r=[self.XT], w=[o])
        if "stopn_%d" % l in self.debug:
            return
        if l == 0:
            self.ZT = p.dram([T // 128, 128, KC, 128], BF16, "ZT")
        if l % 2 == 0:
            self.phase_hy_inproj(l)
            self.phase_hy_conv(l)
            if "zt_%d" % l in self.debug:
                o = self.outp("dbg_zt_%d" % l, [T // 128, 128, KC, 128], BF16)
                p.dma(o.ap(), self.ZT.ap(), r=[self.ZT], w=[o])
            self.phase_outproj(l, self.ZT, self.W["hy_w_out"], self.hy["hy_b_out"], 2, tiles_x, tiles_c if not last else [])
        if l % 2 == 1:
            assert last
            self.phase_na_qkv(l)
            self.phase_na_attn(l)
            if "oo_%d" % l in self.debug:
                o = self.outp("dbg_oo_%d" % l, [L, D], BF16)
                p.dma(o.ap(), self.OO.ap(), r=[self.OO], w=[o])
            self.phase_transpose(self.OO, tiles_x, self.ZT)
            self.phase_outproj(l, self.ZT, self.W["na_w_out"], None, 2, tiles_x, [])
        if "hx1_%d" % l in self.debug:
            o = self.outp("dbg_hx1_%d" % l, [T, D], F32)
            p.dma(o.ap(), self.HX.ap(), r=[self.HX], w=[o])
        if "stop1_%d" % l in self.debug:
            return
        tc2 = tiles_c if not last else []
        self.phase_norm(l, 1, "norm2_g", tiles_x, tc2, self.XT)
        if "stopn2_%d" % l in self.debug:
            return
        self.phase_peer(l, tiles_x, tc2)
        if "hx2_%d" % l in self.debug:
            o = self.outp("dbg_hx2_%d" % l, [T, D], F32)
            p.dma(o.ap(), self.HX.ap(), r=[self.HX], w=[o])


def _bf(a):
    return np.ascontiguousarray(a).astype(ml_dtypes.bfloat16)


def dft_mats(L):
    N = 2 * L
    s = np.arange(L, dtype=np.float64)[:, None]
    f = np.arange(L, dtype=np.float64)[None, :]
    ang = 2 * np.pi * f * s / N
    WF = np.zeros((L, N))
    WF[:, :L] = np.cos(ang)
    WF[:, L] = (-1.0) ** np.arange(L)
    WF[:, L + 1:] = -np.sin(ang[:, 1:])
    WI = np.zeros((N, L))
    WI[:L, :] = (2.0 / N) * np.cos(ang.T)
    WI[0, :] = 1.0 / N
    WI[L, :] = (1.0 / N) * (-1.0) ** np.arange(L)
    WI[L + 1:, :] = -(2.0 / N) * np.sin(ang.T[1:, :])
    return WF, WI


def host_inputs(cfg, inputs, core, names):
    D, L, CTX = cfg["D"], cfg["L"], cfg["CTX"]
    b = core % cfg["B"]
    sh = lambda a: np.ascontiguousarray(np.array_split(a, NCORES, axis=0)[core])
    f32 = lambda a: np.ascontiguousarray(np.asarray(a, dtype=np.float32))
    m = {}
    C = _CONST_CACHE.setdefault((L, CTX), {})
    if not C:
        for nm, Lx in (("l", L), ("c", CTX)):
            WF, WI = dft_mats(Lx)
            n = Lx // 128
            C["wf_" + nm] = _bf(WF.reshape(n, 128, 2 * n, 128).transpose(2, 1, 0, 3).reshape(2 * Lx, Lx))
            C["wi_" + nm] = _bf(WI.reshape(2 * n, 128, n, 128).transpose(2, 1, 0, 3).reshape(Lx, 2 * Lx))
        C.update(hyena_consts(cfg))
    m["ident_b"] = _bf(np.eye(128))
    m["ident_f"] = np.eye(128, dtype=np.float32)
    m["x"] = f32(inputs["x"][b])
    m["ctx"] = f32(inputs["ctx"][b])
    for k in ("hy_w_in", "hy_w_out", "na_w_qkv", "na_w_out"):
        m[k] = sh(f32(inputs[k][0]))
    for l in range(cfg["DEPTH"]):
        KCh = D // 128
        wq = f32(inputs["peer_wq"][l])
        NQ = wq.shape[1]
        m["peer_wq%d" % l] = sh(wq.reshape(KCh, 128, NQ // 128, 128).transpose(2, 1, 0, 3).reshape(NQ, D))
        u = f32(inputs["peer_u"][l])
        m["peer_uT%d" % l] = sh(u.reshape(u.shape[0] // 128, 128, KCh, 128).transpose(0, 3, 2, 1).reshape(u.shape[0], D))
        m["peer_k1T%d" % l] = np.ascontiguousarray(f32(inputs["peer_k1"][l]).transpose(2, 0, 1))
        m["peer_k2T%d" % l] = np.ascontiguousarray(f32(inputs["peer_k2"][l]).transpose(2, 0, 1))
        m["peer_v%d" % l] = sh(f32(inputs["peer_v"][l]))
    for k in ("wf_l", "wi_l", "wf_c", "wi_c"):
        m[k] = sh(C[k])
    m["c_all"] = f32(np.concatenate([inputs["c"], inputs["c_ctx"][None]], axis=0))
    aw = f32(inputs["ada_w"]).reshape(cfg["DEPTH"], D, 6, NCORES, D // NCORES)
    m["ada_w"] = np.ascontiguousarray(aw[:, :, :, core, :]).reshape(cfg["DEPTH"], D, 6 * D // NCORES)
    ab = f32(inputs["ada_b"]).reshape(cfg["DEPTH"], 6, NCORES, D // NCORES)
    m["ada_b"] = np.ascontiguousarray(ab[:, :, core, :]).reshape(cfg["DEPTH"], 6 * D // NCORES)
    sx = np.zeros((5, 128), np.float32)
    sx[b] = 1.0
    sc = np.zeros((5, 128), np.float32)
    sc[4] = 1.0
    m["sel_x"], m["sel_c"] = sx, sc
    m["norm1_g"], m["norm2_g"] = f32(inputs["norm1_g"]), f32(inputs["norm2_g"])
    for k in ("hy_b_in", "hy_conv_w", "hy_conv_b", "hy_skip", "hy_b_out", "hy_f_w1", "hy_f_w2", "hy_f_w3", "hy_f_w4"):
        m[k] = f32(inputs[k][0])
    for k in ("hy_f_b1", "hy_f_b2", "hy_f_b3", "hy_f_freq"):
        m[k] = f32(inputs[k][0]).reshape(-1, 1)
    for k in ("zT_l", "zT_c", "negt_l", "negt_c", "deltas"):
        m[k] = C[k]
    m["na_q_g"], m["na_k_g"] = f32(inputs["na_q_g"][0]), f32(inputs["na_k_g"][0])
    m["na_rpbx"], m["na_maskT"] = na_consts(cfg, f32(inputs["na_rpb"][0]))
    return {k: m[k] for k in names}


_CONST_CACHE = {}


def run(cfg, inputs, debug=()):
    bld = Builder(cfg, debug)
    nc = bld.build()
    names = list(bld.inputs.keys())
    in_maps = [host_inputs(cfg, inputs, c, names) for c in range(NCORES)]
    for m in in_maps:
        for k, (shape, dt) in bld.inputs.items():
            assert tuple(m[k].shape) == tuple(shape), (k, m[k].shape, shape)
    res = run_bass_kernel_spmd(nc, in_maps, core_ids=list(range(NCORES)))
    return res, bld


def kernel(**inputs):
    cfg = CFG_FULL
    res, bld = run(cfg, inputs)
    out = np.stack([np.asarray(res.results[b]["out"], dtype=np.float32) for b in range(cfg["B"])], axis=0)
    return out


def phase_proj(self, XT, tiles, Wfull, N, evac, ncol=512):
    cfg, p = self.cfg, self.p
    KC = self.KC
    with ExitStack() as ph:
        wt = [p.sb([128, KC, ncol], BF16, "pw", es=ph) for _ in range(2)]
        xt = [p.sb([128, KC, 128], BF16, "px", es=ph) for _ in range(3)]
        self.proj_ph = ph
        it = 0
        Wv = Wfull.ap().rearrange("(k p) n -> p k n", p=128)
        for ci, c0 in enumerate(range(0, N, ncol)):
            c1 = min(N, c0 + ncol)
            w = wt[ci % 2]
            p.dma(w[:, :, 0:c1 - c0], Wv[:, :, c0:c1], r=[Wfull], w=[w])
            for j in tiles:
                x_ = xt[it % 3]
                it += 1
                p.dma(x_[:], XT[j, :, :, :], r=[XT], w=[x_])
                ps = self.next_psf()
                for k in range(KC):
                    p.op("pe", lambda e: e.matmul(ps[:, 0:c1 - c0], lhsT=x_[:, k, :], rhs=w[:, k, 0:c1 - c0],
                                                  start=(k == 0), stop=(k == KC - 1)), r=[x_, w], w=[ps])
                evac(ps, j, c0, c1)
        p.barrier()


Builder.phase_proj = phase_proj


def phase_hy_inproj(self, l):
    cfg, p = self.cfg, self.p
    D, L, CTX, T = cfg["D"], cfg["L"], cfg["CTX"], self.T
    N = 3 * D
    if not hasattr(self, "PP"):
        self.PP = p.dram([T + 4, N], F32, "PP")
        self.hy = {k: self.inp(k, shp) for k, shp in [
            ("hy_b_in", [N]), ("hy_conv_w", [3, N]), ("hy_conv_b", [N]), ("hy_skip", [2, D]), ("hy_b_out", [D]),
            ("hy_f_w1", [cfg["EMB"], cfg["FILT"]]), ("hy_f_b1", [cfg["FILT"], 1]), ("hy_f_w2", [cfg["FILT"], cfg["FILT"]]),
            ("hy_f_b2", [cfg["FILT"], 1]), ("hy_f_w3", [cfg["FILT"], cfg["FILT"]]), ("hy_f_b3", [cfg["FILT"], 1]),
            ("hy_f_w4", [cfg["FILT"], 4 * D]), ("hy_f_freq", [cfg["FILT"], 1]),
            ("zT_l", [cfg["EMB"], L]), ("zT_c", [cfg["EMB"], CTX]), ("negt_l", [128, L // 128]),
            ("negt_c", [128, CTX // 128]), ("deltas", [D])]}
    PP = self.PP
    with ExitStack() as ph:
        zt = p.sb([128, 512], F32, "zero", es=ph)
        p.op("dve", lambda e: e.memset(zt[:], 0.0), w=[zt])
        for r in (0, L + 1, L + 2, L + 3 + CTX):
            for c0 in range(0, N, 512 * 128):
                n = min(N - c0, 512 * 128)
                p.dma(PP[r, c0:c0 + n].rearrange("(a b) -> a b", b=512), zt[0:n // 512, :], r=[zt], w=[PP])
        bias = p.sb([128, N], F32, "hbin", es=ph)
        self.bcast_row(bias, self.hy["hy_b_in"][:], self.hy["hy_b_in"], N)
        ot = [p.sb([128, 512], F32, "po", es=ph) for _ in range(3)]
        cnt = [0]

        def evac(ps, j, c0, c1):
            o = ot[cnt[0] % 3]
            cnt[0] += 1
            p.op("dve", lambda e: e.tensor_tensor(out=o[:, 0:c1 - c0], in0=ps[:, 0:c1 - c0], in1=bias[:, c0:c1], op=ALU.add),
                 r=[ps, bias], w=[o])
            r0 = 1 + j * 128 if j < L // 128 else L + 3 + (j - L // 128) * 128
            p.dma(PP[r0:r0 + 128, c0:c1], o[:, 0:c1 - c0], r=[o], w=[PP])
        self.phase_proj(self.XT, list(range(T // 128)), self.W["hy_w_in"], N, evac)


Builder.phase_hy_inproj = phase_hy_inproj


def phase_hy_conv(self, l):
    cfg, p = self.cfg, self.p
    D, L, CTX, T, KC = cfg["D"], cfg["L"], cfg["CTX"], self.T, self.KC
    FI = cfg["FILT"]
    CT = min(256, D)
    NL, NC_ = L // 128, CTX // 128
    NCH = NL + NC_
    hy, PP = self.hy, self.PP
    segs = [(0, NL, self.W["wf_l"], self.W["wi_l"], 0, "l"), (NL, NC_, self.W["wf_c"], self.W["wi_c"], 2 * NL, "c")]
    PI = math.pi
    with ExitStack() as ph:
        sb = lambda shape, dt, name: p.sb(shape, dt, name, es=ph)
        h3T = sb([FI, T], F32, "h3T")
        ph_outer = ph
        ph = ExitStack()
        sb = lambda shape, dt, name: p.sb(shape, dt, name, es=ph)
        w1 = sb([cfg["EMB"], FI], F32, "fw1")
        w2 = sb([FI, FI], F32, "fw2")
        w3 = sb([FI, FI], F32, "fw3")
        fb = [sb([FI, 1], F32, "fb") for _ in range(3)]
        fr = sb([FI, 1], F32, "ffr")
        p.dma(w1[:], hy["hy_f_w1"][:, :], r=[hy["hy_f_w1"]], w=[w1])
        p.dma(w2[:], hy["hy_f_w2"][:, :], r=[hy["hy_f_w2"]], w=[w2])
        p.dma(w3[:], hy["hy_f_w3"][:, :], r=[hy["hy_f_w3"]], w=[w3])
        for i, k in enumerate(("hy_f_b1", "hy_f_b2", "hy_f_b3")):
            p.dma(fb[i][:], hy[k][:, :], r=[hy[k]], w=[fb[i]])
        p.dma(fr[:], hy["hy_f_freq"][:, :], r=[hy["hy_f_freq"]], w=[fr])
        zT = sb([cfg["EMB"], T], F32, "zT")
        p.dma(zT[:, 0:L], hy["zT_l"][:, :], r=[hy["zT_l"]], w=[zT])
        p.dma(zT[:, L:T], hy["zT_c"][:, :], r=[hy["zT_c"]], w=[zT])
        hA = sb([FI, T], F32, "hA")
        hB = sb([FI, T], F32, "hB")
        arg = sb([FI, 512], F32, "farg")
        argm = sb([FI, 512], F32, "fargm")
        for li, (wl, src, dst) in enumerate(((w1, zT, hA), (w2, hA, hB), (w3, hB, h3T))):
            for n0 in range(0, T, 512):
                n1 = min(T, n0 + 512)
                ps = self.next_psf()
                p.op("pe", lambda e: e.matmul(ps[0:FI, 0:n1 - n0], lhsT=wl[:], rhs=src[:, n0:n1], start=True, stop=True),
                     r=[wl, src], w=[ps])
                p.op("dve", lambda e: e.tensor_scalar(out=arg[:, 0:n1 - n0], in0=ps[0:FI, 0:n1 - n0], scalar1=fb[li][:, 0:1],
                                                      scalar2=fr[:, 0:1], op0=ALU.add, op1=ALU.mult), r=[ps, fb[li], fr], w=[arg])
                for _ in range(2):
                    for thr, cmp, add in ((PI, ALU.is_gt, -2 * PI), (-PI, ALU.is_lt, 2 * PI)):
                        p.op("dve", lambda e: e.tensor_scalar(out=argm[:, 0:n1 - n0], in0=arg[:, 0:n1 - n0], scalar1=thr, scalar2=add,
                                                              op0=cmp, op1=ALU.mult), r=[arg], w=[argm])
                        p.op("dve", lambda e: e.tensor_tensor(out=arg[:, 0:n1 - n0], in0=arg[:, 0:n1 - n0], in1=argm[:, 0:n1 - n0],
                                                              op=ALU.add), r=[arg, argm], w=[arg])
                p.op("act", lambda e: e.activation(out=dst[:, n0:n1], in_=arg[:, 0:n1 - n0], func=AF.Sin), r=[arg], w=[dst])
        p.barrier()
        ph.close()
        ph = ph_outer
        sb = lambda shape, dt, name: p.sb(shape, dt, name, es=ph)
        negt = sb([128, NCH], F32, "negt")
        p.dma(negt[:, 0:NL], hy["negt_l"][:, :], r=[hy["negt_l"]], w=[negt])
        p.dma(negt[:, NL:NCH], hy["negt_c"][:, :], r=[hy["negt_c"]], w=[negt])
        tmp = [sb([128, NCH, CT], F32, "ctmp")] * 2
        V = sb([128, NCH, CT], F32, "cV")
        X = sb([128, NCH, CT], F32, "cX")
        Ub = sb([128, NCH, CT], BF16, "cUb")
        Y = sb([128, 2 * NCH, CT], BF16, "cY")
        gp = sb([128, NCH, CT], BF16, "cgp")
        gm = sb([128, NCH, CT], BF16, "cgm")
        KA = sb([128, NCH, CT], BF16, "cKA")
        KB = sb([128, NCH, CT], BF16, "cKB")
        KD0 = [sb([128, CT], BF16, "cKD0") for _ in range(2)]
        wfs = [sb([128, NL, 128], BF16, "wfs") for _ in range(2)]
        wis = [sb([128, 2 * NL, 128], BF16, "wis") for _ in range(2)]
        rows = {k: sb([128, CT], F32, "crow_" + k) for k in
                ["w0", "w1", "w2", "cb", "sk0", "sk1", "dl"]}
        w4t = sb([FI, 4, CT], F32, "w4t")
        wn = [sb([128, CT], F32, "cwn") for _ in range(2)]
        sm = [sb([128, CT], F32, "csm") for _ in range(6)]
        rns = [sb([128, CT], F32, "crn") for _ in range(2)]
        zT_ = sb([128, CT // 128, T], BF16, "czT")
        self.slab_i = 0
        self.sm_i = 0
        ZTv = self.ZT.ap().rearrange("j p k t -> p k j t")

        def bc(t):
            return t[:].unsqueeze(1).broadcast_to([128, NCH, CT])

        def short_conv(g, c0, dst):
            col = g * D + c0
            for k, src in (("w0", hy["hy_conv_w"][0, col:col + CT]), ("w1", hy["hy_conv_w"][1, col:col + CT]),
                           ("w2", hy["hy_conv_w"][2, col:col + CT]), ("cb", hy["hy_conv_b"][col:col + CT])):
                p.dma(rows[k][:], src.partition_broadcast(128), r=[hy["hy_conv_w"], hy["hy_conv_b"]], w=[rows[k]])

            def load(tap, t_):
                p.dma(t_[:, 0:NL, :], PP[tap:tap + L, col:col + CT].rearrange("(j q) c -> q j c", q=128), r=[PP], w=[t_])
                p.dma(t_[:, NL:NCH, :], PP[L + 2 + tap:L + 2 + tap + CTX, col:col + CT].rearrange("(j q) c -> q j c", q=128),
                      r=[PP], w=[t_])
            load(0, tmp[0])
            p.op("pool", lambda e: e.tensor_tensor(out=dst[:], in0=tmp[0][:], in1=bc(rows["w0"]), op=ALU.mult),
                 r=[tmp[0], rows["w0"]], w=[dst])
            p.op("dve", lambda e: e.tensor_tensor(out=dst[:], in0=dst[:], in1=bc(rows["cb"]), op=ALU.add),
                 r=[dst, rows["cb"]], w=[dst])
            for tap in (1, 2):
                t_ = tmp[tap % 2]
                load(tap, t_)
                p.op("pool", lambda e: e.tensor_tensor(out=t_[:], in0=t_[:], in1=bc(rows["w%d" % tap]), op=ALU.mult),
                     r=[t_, rows["w%d" % tap]], w=[t_])
                p.op("dve", lambda e: e.tensor_tensor(out=dst[:], in0=dst[:], in1=t_[:], op=ALU.add), r=[dst, t_], w=[dst])

        def fwd(U, seg, parts, cb):
            off, n, WF, WI, yoff, nm = seg
            for kind, j in parts:
                ic = j if kind == "re" else n + j
                slab = wfs[self.slab_i % 2]
                self.slab_i += 1
                p.dma(slab[:, 0:n, :], WF[ic * 128:(ic + 1) * 128, :].rearrange("q (s i) -> q s i", i=128), r=[WF], w=[slab])
                ps = self.next_psf()
                for s in range(n):
                    p.op("pe", lambda e: e.matmul(ps[:, 0:CT], lhsT=slab[:, s, :], rhs=U[:, off + s, :], start=(s == 0),
                                                  stop=(s == n - 1)), r=[slab, U], w=[ps])
                cb(ps, kind, j)

        def smt():
            t = sm[self.sm_i % 6]
            self.sm_i += 1
            return t

        def filters(o, c0):
            for dr in range(2):
                col = dr * 2 * D + o * D + c0
                p.dma(w4t[:, dr, :], hy["hy_f_w4"][:, col:col + CT], r=[hy["hy_f_w4"]], w=[w4t])
            for seg in segs:
                off, n = seg[0], seg[1]
                si = 0 if seg[5] == "l" else 1
                rn = rns[si]
                psn = self.psx
                for j in range(n):
                    w_ = wn[j % 2]
                    p.op("act", lambda e: e.activation(out=w_[:], in_=rows["dl"][:], func=AF.Exp,
                                                       scale=negt[:, off + j:off + j + 1]), r=[rows["dl"], negt], w=[w_])
                    fts = []
                    for dr in range(2):
                        ps = self.next_psf()
                        p.op("pe", lambda e: e.matmul(ps[:, 0:CT], lhsT=h3T[:, (off + j) * 128:(off + j + 1) * 128],
                                                      rhs=w4t[:, dr, :], start=True, stop=True), r=[h3T, w4t], w=[ps])
                        f = smt()
                        p.op("dve", lambda e: e.scalar_tensor_tensor(out=f[:], in0=w_[:], scalar=cfg_shift, in1=ps[:, 0:CT],
                                                                     op0=ALU.add, op1=ALU.mult), r=[w_, ps], w=[f])
                        if dr == 1 and j == 0:
                            p.op("dve", lambda e: e.memset(f[0:1, :], 0.0), w=[f])
                        a_ = smt()
                        p.op("act", lambda e: e.activation(out=a_[:], in_=f[:], func=AF.Abs), r=[f], w=[a_])
                        first = (dr == 0 and j == 0)
                        lastm = (dr == 1 and j == n - 1)
                        p.op("pe", lambda e: e.matmul(psn[:, 0:CT], lhsT=self.ones_f[:], rhs=a_[:], start=first, stop=lastm),
                             r=[self.ones_f, a_], w=[psn])
                        fts.append(f)
                    p.op("pool", lambda e: e.tensor_tensor(out=gp[:, off + j, :], in0=fts[0][:], in1=fts[1][:], op=ALU.add),
                         r=[fts[0], fts[1]], w=[gp])
                    p.op("pool", lambda e: e.tensor_tensor(out=gm[:, off + j, :], in0=fts[0][:], in1=fts[1][:], op=ALU.subtract),
                         r=[fts[0], fts[1]], w=[gm])
                p.op("dve", lambda e: e.reciprocal(out=rn[:], in_=psn[:, 0:CT]), r=[psn], w=[rn])

                def cbA(ps, kind, j):
                    p.op("dve", lambda e: e.tensor_tensor(out=KA[:, off + j, :], in0=ps[:, 0:CT], in1=rn[:], op=ALU.mult), r=[ps, rn], w=[KA])

                def cbB(ps, kind, j):
                    p.op("dve", lambda e: e.tensor_tensor(out=KB[:, off + j, :], in0=ps[:, 0:CT], in1=rn[:], op=ALU.mult), r=[ps, rn], w=[KB])

                def cbN(ps, kind, j):
                    p.op("act", lambda e: e.copy(out=KD0[si][:], in_=KA[:, off, :]), r=[KA], w=[KD0[si]])
                    p.op("dve", lambda e: e.tensor_tensor(out=KD0[si][0:1, :], in0=ps[0:1, 0:CT], in1=rn[0:1, :], op=ALU.mult),
                         r=[ps, rn], w=[KD0[si]])
                fwd(gp, seg, [("re", j) for j in range(n)], cbA)
                fwd(gm, seg, [("im", j) for j in range(n)], cbB)
                fwd(gp, seg, [("im", 0)], cbN)
                p.op("dve", lambda e: e.memset(KB[0:1, off, :], 0.0), w=[KB])

        def spectrum_mul(U, seg):
            off, n, WF, WI, yoff, nm = seg
            si = 0 if nm == "l" else 1
            hold = {}

            def cb(ps, kind, j):
                u = smt()
                p.op("act", lambda e: e.copy(out=u[:], in_=ps[:, 0:CT]), r=[ps], w=[u])
                hold[kind] = u
                if kind == "im":
                    ure, uim = hold["re"], hold["im"]
                    t1, t2, t3, t4 = smt(), smt(), smt(), smt()
                    kd = KD0[si] if j == 0 else None
                    p.op("dve", lambda e: e.tensor_tensor(out=t1[:], in0=ure[:], in1=KA[:, off + j, :], op=ALU.mult), r=[ure, KA], w=[t1])
                    p.op("pool", lambda e: e.tensor_tensor(out=t2[:], in0=uim[:], in1=KB[:, off + j, :], op=ALU.mult), r=[uim, KB], w=[t2])
                    p.op("dve", lambda e: e.tensor_tensor(out=Y[:, yoff + j, :], in0=t1[:], in1=t2[:], op=ALU.subtract), r=[t1, t2], w=[Y])
                    p.op("pool", lambda e: e.tensor_tensor(out=t3[:], in0=ure[:], in1=KB[:, off + j, :], op=ALU.mult), r=[ure, KB], w=[t3])
                    if kd is not None:
                        p.op("dve", lambda e: e.tensor_tensor(out=t4[:], in0=uim[:], in1=kd[:], op=ALU.mult), r=[uim, kd], w=[t4])
                    else:
                        p.op("dve", lambda e: e.tensor_tensor(out=t4[:], in0=uim[:], in1=KA[:, off + j, :], op=ALU.mult), r=[uim, KA], w=[t4])
                    p.op("pool", lambda e: e.tensor_tensor(out=Y[:, yoff + n + j, :], in0=t3[:], in1=t4[:], op=ALU.add), r=[t3, t4], w=[Y])
            parts = []
            for j in range(n):
                parts += [("re", j), ("im", j)]
            fwd(U, seg, parts, cb)

        def inverse(seg, cb):
            off, n, WF, WI, yoff, nm = seg
            for tc in range(n):
                slab = wis[self.slab_i % 2]
                self.slab_i += 1
                p.dma(slab[:, 0:2 * n, :], WI[tc * 128:(tc + 1) * 128, :].rearrange("q (s i) -> q s i", i=128), r=[WI], w=[slab])
                ps = self.next_psf()
                for ic in range(2 * n):
                    p.op("pe", lambda e: e.matmul(ps[:, 0:CT], lhsT=slab[:, ic, :], rhs=Y[:, yoff + ic, :], start=(ic == 0),
                                                  stop=(ic == 2 * n - 1)), r=[slab, Y], w=[ps])
                cb(ps, off + tc)

        cfg_shift = 0.05
        for c0 in range(0, D, CT):
            p.dma(rows["dl"][:], hy["deltas"][c0:c0 + CT].partition_broadcast(128), r=[hy["deltas"]], w=[rows["dl"]])
            p.dma(rows["sk0"][:], hy["hy_skip"][0, c0:c0 + CT].partition_broadcast(128), r=[hy["hy_skip"]], w=[rows["sk0"]])
            p.dma(rows["sk1"][:], hy["hy_skip"][1, c0:c0 + CT].partition_broadcast(128), r=[hy["hy_skip"]], w=[rows["sk1"]])
            short_conv(0, c0, V)
            p.op("act", lambda e: e.copy(out=Ub[:], in_=V[:]), r=[V], w=[Ub])
            for o in range(2):
                filters(o, c0)
                for seg in segs:
                    spectrum_mul(Ub, seg)
                short_conv(1 + o, c0, X)
                sk = rows["sk%d" % o]

                def cb(ps, ch):
                    t1 = smt()
                    p.op("pool", lambda e: e.tensor_tensor(out=t1[:], in0=V[:, ch, :], in1=sk[:], op=ALU.mult), r=[V, sk], w=[t1])
                    p.op("dve", lambda e: e.tensor_tensor(out=t1[:], in0=t1[:], in1=ps[:, 0:CT], op=ALU.add), r=[t1, ps], w=[t1])
                    p.op("pool", lambda e: e.tensor_tensor(out=V[:, ch, :], in0=t1[:], in1=X[:, ch, :], op=ALU.mult), r=[t1, X], w=[V])
                for seg in segs:
                    inverse(seg, cb)
                p.op("act", lambda e: e.copy(out=Ub[:], in_=V[:]), r=[V], w=[Ub])
            for kk in range(CT // 128):
                for j0 in range(0, NCH, 8):
                    jn = min(8, NCH - j0)
                    pb = self.next_psb()
                    for j in range(j0, j0 + jn):
                        p.op("pe", lambda e: e.transpose(out=pb[:, (j - j0) * 128:(j - j0 + 1) * 128],
                                                         in_=Ub[:, j, kk * 128:(kk + 1) * 128], identity=self.ident_b[:]),
                             r=[Ub, self.ident_b], w=[pb])
                    p.op("act", lambda e: e.copy(out=zT_[:, kk, j0 * 128:(j0 + jn) * 128], in_=pb[:, 0:jn * 128]), r=[pb], w=[zT_])
                k = c0 // 128 + kk
                p.dma(ZTv[:, k, :, :], zT_[:, kk, :].rearrange("q (j t) -> q j t", t=128), r=[zT_], w=[self.ZT])
        p.barrier()


Builder.phase_hy_conv = phase_hy_conv


def phase_outproj(self, l, XTsrc, Wfull, bias_d, gate_chunk, tiles_x, tiles_c):
    cfg, p = self.cfg, self.p
    D, L = cfg["D"], cfg["L"]
    with ExitStack() as ph:
        self.modrows = p.sb([5, D], F32, "modrows", es=ph)
        gate = {}
        for stream, tiles in (("x", tiles_x), ("c", tiles_c)):
            if tiles:
                gate[stream] = p.sb([128, D], F32, "gate", es=ph)
                self.mod_tile(gate[stream], l, gate_chunk, stream)
        bias = None
        if bias_d is not None:
            bias = p.sb([128, D], F32, "obias", es=ph)
            self.bcast_row(bias, bias_d[:], bias_d, D)
        ht = [p.sb([128, 512], F32, "oh", es=ph) for _ in range(3)]
        tt = [p.sb([128, 512], F32, "ot", es=ph) for _ in range(3)]
        cnt = [0]

        def evac(ps, j, c0, c1):
            n = c1 - c0
            h_, t_ = ht[cnt[0] % 3], tt[cnt[0] % 3]
            cnt[0] += 1
            g = gate["x" if j < L // 128 else "c"]
            p.dma(h_[:, 0:n], self.HX[j * 128:(j + 1) * 128, c0:c1], r=[self.HX], w=[h_])
            if bias is not None:
                p.op("dve", lambda e: e.tensor_tensor(out=t_[:, 0:n], in0=ps[:, 0:n], in1=bias[:, c0:c1], op=ALU.add),
                     r=[ps, bias], w=[t_])
                p.op("pool", lambda e: e.tensor_tensor(out=t_[:, 0:n], in0=t_[:, 0:n], in1=g[:, c0:c1], op=ALU.mult),
                     r=[t_, g], w=[t_])
            else:
                p.op("dve", lambda e: e.tensor_tensor(out=t_[:, 0:n], in0=ps[:, 0:n], in1=g[:, c0:c1], op=ALU.mult),
                     r=[ps, g], w=[t_])
            p.op("dve", lambda e: e.tensor_tensor(out=h_[:, 0:n], in0=h_[:, 0:n], in1=t_[:, 0:n], op=ALU.add),
                 r=[h_, t_], w=[h_])
            p.dma(self.HX[j * 128:(j + 1) * 128, c0:c1], h_[:, 0:n], r=[h_], w=[self.HX])
        self.phase_proj(XTsrc, list(tiles_x) + list(tiles_c), Wfull, D, evac)


Builder.phase_outproj = phase_outproj


def hyena_consts(cfg):
    out = {}
    for nm, Lx in (("l", cfg["L"]), ("c", cfg["CTX"])):
        t = np.linspace(0.0, 1.0, Lx, dtype=np.float32)
        wpos = (np.float32(2.0 * math.pi / Lx) * np.arange(Lx, dtype=np.float32))
        nb = (cfg["EMB"] - 1) // 2
        bands = np.linspace(1e-4, nb - 1, nb, dtype=np.float32)
        z = np.concatenate([t[:, None], np.cos(bands[None, :] * wpos[:, None]), -np.sin(bands[None, :] * wpos[:, None])], axis=-1)
        out["zT_" + nm] = np.ascontiguousarray(z.T.astype(np.float32))
        out["negt_" + nm] = np.ascontiguousarray((-t).reshape(Lx // 128, 128).T.astype(np.float32))
    max_decay = math.log(1e-2) / 0.3
    min_decay = math.log(1e-2) / 1.5
    out["deltas"] = np.abs(np.linspace(min_decay, max_decay, cfg["D"], dtype=np.float32)).astype(np.float32)
    return out


def phase_peer(self, l, tiles_x, tiles_c):
    cfg, p = self.cfg, self.p
    D, L, KC = cfg["D"], cfg["L"], self.KC
    PH, PK, TOPK = cfg["PH"], cfg["PK"], cfg["TOPK"]
    E = PK * PK
    EC = E // 128
    EG = 8
    NG = EC // EG
    TG = 2
    NEG = -1.0e30
    if not hasattr(self, "pk"):
        self.pk = {}
        for ll in range(cfg["DEPTH"]):
            self.pk[ll] = (self.inp("peer_k1T%d" % ll, [128, PH, PK]), self.inp("peer_k2T%d" % ll, [128, PH, PK]))
    wq, uT, vv = self.W["peer_wq%d" % l], self.W["peer_uT%d" % l], self.W["peer_v%d" % l]
    vview = vv.ap().rearrange("(g c q) d -> g q c d", c=EG, q=128)
    tiles = list(tiles_x) + list(tiles_c)
    gate_d = {}
    with ExitStack() as ph0:
        self.modrows = p.sb([5, D], F32, "modrows", es=ph0)
        gtmp = p.sb([128, D], F32, "pgtmp", es=ph0)
        for stream, tl in (("x", tiles_x), ("c", tiles_c)):
            if tl:
                gate_d[stream] = p.dram([128, D], F32, "peer_gate_%d_%s" % (l, stream))
                self.mod_tile(gtmp, l, 5, stream)
                p.dma(gate_d[stream].ap(), gtmp[:], r=[gtmp], w=[gate_d[stream]])
        p.barrier()
    with ExitStack() as ph:
        sb = lambda shape, dt, name: p.sb(shape, dt, name, es=ph)
        gate1 = sb([128, D], F32, "pgate")
        gate_stream = [None]

        def set_gate(stream):
            if gate_stream[0] == stream:
                return
            gate_stream[0] = stream
            p.dma(gate1[:], gate_d[stream].ap(), r=[gate_d[stream]], w=[gate1])
        k1T = sb([128, PH, PK], F32, "k1T")
        k2T = sb([128, PH, PK], F32, "k2T")
        p.dma(k1T[:], self.pk[l][0].ap(), r=[self.pk[l][0]], w=[k1T])
        p.dma(k2T[:], self.pk[l][1].ap(), r=[self.pk[l][1]], w=[k2T])
        xg = sb([128, KC, TG * 128], BF16, "pxg")
        slab = [sb([128, KC, 128], BF16, "pslab") for _ in range(2)]
        qT = sb([128, 2 * PH, TG * 128], F32, "pqT")
        s1 = [sb([128, PH, PK], F32, "ps1") for _ in range(TG)]
        s2 = [sb([128, PH, PK], F32, "ps2") for _ in range(TG)]
        thr = [sb([128, PH], F32, "pthr") for _ in range(TG)]
        negb = [sb([128, PH], F32, "pnegb") for _ in range(TG)]
        acc = [sb([128, D], F32, "pacc") for _ in range(TG)]
        v1 = sb([128, TOPK], F32, "pv1")
        v2 = sb([128, TOPK], F32, "pv2")
        wk = sb([128, PK], F32, "pwk")
        cand = sb([128, TOPK * TOPK], F32, "pcand")
        cand2 = sb([128, TOPK * TOPK], F32, "pcand2")
        c16 = sb([128, TOPK], F32, "pc16")
        sc = [sb([128, 1], F32, "psc") for _ in range(4)]
        Tb = [sb([128, EG * 128], F32, "pT") for _ in range(2)]
        Eb = [sb([128, EG * 128], F32, "pE") for _ in range(2)]
        Cb = [sb([128, EG * 128], F32, "pC") for _ in range(2)]
        Gs = sb([128, EG * 128], F32, "pGs")
        Gb = sb([128, EG * 128], BF16, "pGb")
        gA = sb([128, EG, TG * 128], BF16, "pgA")
        CT = sb([128, EG, TG * 128], BF16, "pCT")
        Vt = [sb([128, EG, 512], BF16, "pVt") for _ in range(2)]
        hxs = [sb([128, 512], F32, "phx") for _ in range(2)]
        it = {"slab": 0, "blk": 0, "v": 0}

        def next_slab():
            s_ = slab[it["slab"] % 2]
            it["slab"] += 1
            return s_

        def top16(src_ap, src_buf, dst, n):
            work = wk if n == PK else cand2
            p.op("dve", lambda e: e.max(out=dst[:, 0:8], in_=src_ap), r=[src_buf], w=[dst])
            p.op("dve", lambda e: e.match_replace(out=work[:, 0:n], in_to_replace=dst[:, 0:8], in_values=src_ap, imm_value=NEG),
                 r=[src_buf, dst], w=[work])
            p.op("dve", lambda e: e.max(out=dst[:, 8:16], in_=work[:, 0:n]), r=[work], w=[dst])

        for g0 in range(0, len(tiles), TG):
            grp = tiles[g0:g0 + TG]
            ntok = len(grp) * 128
            for ti, j in enumerate(grp):
                p.dma(xg[:, :, ti * 128:(ti + 1) * 128], self.XT[j, :, :, :], r=[self.XT], w=[xg])
            for cc in range(2 * PH):
                s_ = next_slab()
                p.dma(s_[:], wq[cc * 128:(cc + 1) * 128, :].rearrange("q (k c) -> q k c", c=128), r=[wq], w=[s_])
                ps = self.next_psf()
                for k in range(KC):
                    p.op("pe", lambda e: e.matmul(ps[:, 0:ntok], lhsT=s_[:, k, :], rhs=xg[:, k, 0:ntok], start=(k == 0),
                                                  stop=(k == KC - 1)), r=[s_, xg], w=[ps])
                p.op("act", lambda e: e.copy(out=qT[:, cc, 0:ntok], in_=ps[:, 0:ntok]), r=[ps], w=[qT])
            for ti in range(len(grp)):
                for half, (kT, sdst) in enumerate(((k1T, s1[ti]), (k2T, s2[ti]))):
                    for h0 in range(0, PH, 4):
                        ps = self.next_psf()
                        for h in range(h0, h0 + 4):
                            p.op("pe", lambda e: e.matmul(ps[:, (h - h0) * PK:(h - h0 + 1) * PK],
                                                          lhsT=qT[:, 2 * h + half, ti * 128:(ti + 1) * 128], rhs=kT[:, h, :],
                                                          start=True, stop=True), r=[qT, kT], w=[ps])
                        p.op("act", lambda e: e.copy(out=sdst[:, h0:h0 + 4, :].rearrange("q h k -> q (h k)"), in_=ps[:, 0:4 * PK]),
                             r=[ps], w=[sdst])
                if "pk_notopk" in self.debug:
                    p.op("dve", lambda e: e.memset(thr[ti][:], 0.0), w=[thr[ti]])
                    p.op("dve", lambda e: e.memset(negb[ti][:], 0.0), w=[negb[ti]])
                for h in range(PH if "pk_notopk" not in self.debug else 0):
                    top16(s1[ti][:, h, :], s1[ti], v1, PK)
                    top16(s2[ti][:, h, :], s2[ti], v2, PK)
                    p.op("dve", lambda e: e.tensor_tensor(out=cand[:].rearrange("q (a b) -> q a b", b=TOPK),
                                                          in0=v1[:].unsqueeze(2).broadcast_to([128, TOPK, TOPK]),
                                                          in1=v2[:].unsqueeze(1).broadcast_to([128, TOPK, TOPK]), op=ALU.add),
                         r=[v1, v2], w=[cand])
                    top16(cand[:], cand, c16, TOPK * TOPK)
                    p.op("act", lambda e: e.copy(out=thr[ti][:, h:h + 1], in_=c16[:, 15:16]), r=[c16], w=[thr[ti]])
                    p.op("dve", lambda e: e.tensor_scalar(out=sc[0][:], in0=c16[:, 0:1], scalar1=-1.0, scalar2=None, op0=ALU.mult),
                         r=[c16], w=[sc[0]])
                    p.op("act", lambda e: e.activation(out=v1[:], in_=c16[:], func=AF.Exp, bias=sc[0][:, 0:1], accum_out=sc[1][:]),
                         r=[c16, sc[0]], w=[v1, sc[1]])
                    p.op("act", lambda e: e.activation(out=sc[2][:], in_=sc[1][:], func=AF.Ln), r=[sc[1]], w=[sc[2]])
                    p.op("dve", lambda e: e.tensor_tensor(out=negb[ti][:, h:h + 1], in0=sc[0][:], in1=sc[2][:], op=ALU.subtract),
                         r=[sc[0], sc[2]], w=[negb[ti]])
            for g in range(NG if "pk_noeg" not in self.debug else 0):
                for ci in range(EG):
                    ec = g * EG + ci
                    s_ = next_slab()
                    p.dma(s_[:], uT[ec * 128:(ec + 1) * 128, :].rearrange("q (k c) -> q k c", c=128), r=[uT], w=[s_])
                    ps = self.next_psf()
                    for k in range(KC):
                        p.op("pe", lambda e: e.matmul(ps[:, 0:ntok], lhsT=s_[:, k, :], rhs=xg[:, k, 0:ntok], start=(k == 0),
                                                      stop=(k == KC - 1)), r=[s_, xg], w=[ps])
                    p.op("act", lambda e: e.activation(out=gA[:, ci, 0:ntok], in_=ps[:, 0:ntok], func=AF.Gelu), r=[ps], w=[gA])
                for ti in range(len(grp)):
                    for h in range(PH):
                        b = it["blk"] % 2
                        it["blk"] += 1
                        T_, E_, C_ = Tb[b], Eb[b], Cb[b]
                        p.op("pool", lambda e: e.tensor_tensor(
                            out=T_[:].rearrange("q (a b) -> q a b", b=PK),
                            in0=s1[ti][:, h, g * EG:(g + 1) * EG].unsqueeze(2).broadcast_to([128, EG, PK]),
                            in1=s2[ti][:, h, :].unsqueeze(1).broadcast_to([128, EG, PK]), op=ALU.add),
                            r=[s1[ti], s2[ti]], w=[T_])
                        p.op("act", lambda e: e.activation(out=E_[:], in_=T_[:], func=AF.Exp, bias=negb[ti][:, h:h + 1]),
                             r=[T_, negb[ti]], w=[E_])
                        dst = Gs if h == 0 else C_
                        p.op("dve", lambda e: e.scalar_tensor_tensor(out=dst[:], in0=T_[:], scalar=thr[ti][:, h:h + 1], in1=E_[:],
                                                                     op0=ALU.is_ge, op1=ALU.mult), r=[T_, thr[ti], E_], w=[dst])
                        if h > 0:
                            p.op("pool", lambda e: e.tensor_tensor(out=Gs[:], in0=Gs[:], in1=C_[:], op=ALU.add), r=[Gs, C_], w=[Gs])
                    p.op("act", lambda e: e.copy(out=Gb[:], in_=Gs[:]), r=[Gs], w=[Gb])
                    pb = self.next_psb()
                    for ci in range(EG):
                        p.op("pe", lambda e: e.transpose(out=pb[:, ci * 128:(ci + 1) * 128], in_=Gb[:, ci * 128:(ci + 1) * 128],
                                                         identity=self.ident_b[:]), r=[Gb, self.ident_b], w=[pb])
                    p.op("dve", lambda e: e.tensor_tensor(out=CT[:, :, ti * 128:(ti + 1) * 128],
                                                          in0=pb[:, 0:EG * 128].rearrange("q (c t) -> q c t", t=128),
                                                          in1=gA[:, :, ti * 128:(ti + 1) * 128], op=ALU.mult), r=[pb, gA], w=[CT])
                for dt in range(0, D, 512):
                    dn = min(512, D - dt)
                    V_ = Vt[it["v"] % 2]
                    it["v"] += 1
                    p.dma(V_[:, :, 0:dn], vview[g, :, :, dt:dt + dn], r=[vv], w=[V_])
                    for ti in range(len(grp)):
                        ps = self.next_psf()
                        for ci in range(EG):
                            p.op("pe", lambda e: e.matmul(ps[:, 0:dn], lhsT=CT[:, ci, ti * 128:(ti + 1) * 128], rhs=V_[:, ci, 0:dn],
                                                          start=(ci == 0), stop=(ci == EG - 1)), r=[CT, V_], w=[ps])
                        if g == 0:
                            p.op("act", lambda e: e.copy(out=acc[ti][:, dt:dt + dn], in_=ps[:, 0:dn]), r=[ps], w=[acc[ti]])
                        else:
                            p.op("dve", lambda e: e.tensor_tensor(out=acc[ti][:, dt:dt + dn], in0=acc[ti][:, dt:dt + dn],
                                                                  in1=ps[:, 0:dn], op=ALU.add), r=[acc[ti], ps], w=[acc[ti]])
            for ti, j in enumerate(grp):
                if "pk_noeg" in self.debug:
                    p.op("dve", lambda e: e.memset(acc[ti][:], 0.0), w=[acc[ti]])
                set_gate("x" if j < L // 128 else "c")
                gt_ = gate1
                p.op("pool", lambda e: e.tensor_tensor(out=acc[ti][:], in0=acc[ti][:], in1=gt_[:], op=ALU.mult), r=[acc[ti], gt_], w=[acc[ti]])
                for ci_, c0 in enumerate(range(0, D, 512)):
                    hx = hxs[ci_ % 2]
                    p.dma(hx[:], self.HX[j * 128:(j + 1) * 128, c0:c0 + 512], r=[self.HX], w=[hx])
                    p.op("dve", lambda e: e.tensor_tensor(out=hx[:], in0=hx[:], in1=acc[ti][:, c0:c0 + 512], op=ALU.add), r=[hx, acc[ti]], w=[hx])
                    p.dma(self.HX[j * 128:(j + 1) * 128, c0:c0 + 512], hx[:], r=[hx], w=[self.HX])
        p.barrier()


Builder.phase_peer = phase_peer


def phase_na_qkv(self, l):
    cfg, p = self.cfg, self.p
    D, L, CTX, T, HD = cfg["D"], cfg["L"], cfg["CTX"], self.T, cfg["HD"]
    if not hasattr(self, "QK"):
        self.QK = p.dram([T, 2 * D], BF16, "QK")
        self.VV = p.dram([T, D], BF16, "VV")
        self.OO = p.dram([L, D], BF16, "OO")
        self.na = {"q_g": self.inp("na_q_g", [HD]), "k_g": self.inp("na_k_g", [HD]),
                   "rpbx": self.inp("na_rpbx", [D // HD, 128, 14, cfg["GW"]]), "maskT": self.inp("na_maskT", [128, cfg["GW"]])}
    with ExitStack() as ph:
        gq = p.sb([128, HD], F32, "gq", es=ph)
        gk = p.sb([128, HD], F32, "gk", es=ph)
        self.bcast_row(gq, self.na["q_g"][:], self.na["q_g"], HD)
        self.bcast_row(gk, self.na["k_g"][:], self.na["k_g"], HD)
        p.op("dve", lambda e: e.tensor_scalar(out=gq[:], in0=gq[:], scalar1=float(HD) ** -0.5, scalar2=None, op0=ALU.mult),
             r=[gq], w=[gq])
        sq = [p.sb([128, 512], F32, "qsq", es=ph) for _ in range(2)]
        xs = [p.sb([128, 512], F32, "qxs", es=ph) for _ in range(2)]
        ss = [p.sb([128, 4], F32, "qss", es=ph) for _ in range(2)]
        ob = [p.sb([128, 512], BF16, "qob", es=ph) for _ in range(3)]
        cnt = [0]
        NH = 512 // HD

        def evac(ps, j, c0, c1):
            i = cnt[0]
            cnt[0] += 1
            o = ob[i % 3]
            kind = c0 // D
            if kind == 2:
                p.op("act", lambda e: e.copy(out=o[:], in_=ps[:, 0:512]), r=[ps], w=[o])
                p.dma(self.VV[j * 128:(j + 1) * 128, c0 - 2 * D:c1 - 2 * D], o[:], r=[o], w=[self.VV])
                return
            if kind == 0 and j >= L // 128:
                return
            s_, x_, r_ = sq[i % 2], xs[i % 2], ss[i % 2]
            g = gq if kind == 0 else gk
            p.op("act", lambda e: e.activation(out=s_[:], in_=ps[:, 0:512], func=AF.Square), r=[ps], w=[s_])
            p.op("dve", lambda e: e.tensor_reduce(out=r_[:], in_=s_[:].rearrange("q (h d) -> q h d", d=HD), axis=AX.X, op=ALU.add),
                 r=[s_], w=[r_])
            p.op("dve", lambda e: e.tensor_scalar(out=r_[:], in0=r_[:], scalar1=1.0 / HD, scalar2=1e-6, op0=ALU.mult, op1=ALU.add),
                 r=[r_], w=[r_])
            p.op("act", lambda e: e.activation(out=r_[:], in_=r_[:], func=AF.Sqrt), r=[r_], w=[r_])
            p.op("dve", lambda e: e.reciprocal(out=r_[:], in_=r_[:]), r=[r_], w=[r_])
            p.op("dve", lambda e: e.tensor_tensor(out=x_[:].rearrange("q (h d) -> q h d", d=HD),
                                                  in0=ps[:, 0:512].rearrange("q (h d) -> q h d", d=HD),
                                                  in1=r_[:].unsqueeze(2).broadcast_to([128, NH, HD]), op=ALU.mult), r=[ps, r_], w=[x_])
            p.op("pool", lambda e: e.tensor_tensor(out=o[:].rearrange("q (h d) -> q h d", d=HD),
                                                   in0=x_[:].rearrange("q (h d) -> q h d", d=HD),
                                                   in1=g[:].unsqueeze(1).broadcast_to([128, NH, HD]), op=ALU.mult), r=[x_, g], w=[o])
            p.dma(self.QK[j * 128:(j + 1) * 128, c0:c1], o[:], r=[o], w=[self.QK])
        self.phase_proj(self.XT, list(range(T // 128)), self.W["na_w_qkv"], 3 * D, evac)


Builder.phase_na_qkv = phase_na_qkv


def phase_na_attn(self, l):
    cfg, p = self.cfg, self.p
    D, L, CTX, T, HD, GW = cfg["D"], cfg["L"], cfg["CTX"], self.T, cfg["HD"], cfg["GW"]
    H = D // HD
    R = L // GW
    KH = min(cfg["WIN_H"], R)
    assert KH == 8 and GW == 64 and HD == 128
    NTQ, NTK, NCX = L // 128, T // 128, CTX // 128
    QK, VV, OO = self.QK, self.VV, self.OO
    with ExitStack() as ph:
        sb = lambda shape, dt, name: p.sb(shape, dt, name, es=ph)
        maskT = sb([128, GW], F32, "maskT")
        p.dma(maskT[:], self.na["maskT"][:, :], r=[self.na["maskT"]], w=[maskT])
        qtok = sb([128, NTQ, HD], BF16, "aqtok")
        ktok = sb([128, NTK, HD], BF16, "aktok")
        qT = sb([128, L], BF16, "aqT")
        kT = sb([128, T], BF16, "akT")
        Va = sb([128, NTK, HD + 1], BF16, "aVa")
        Vs = sb([128, NTQ - 1, HD + 1], BF16, "aVs")
        p.op("dve", lambda e: e.memset(Va[:], 1.0), w=[Va])
        p.op("dve", lambda e: e.memset(Vs[:], 1.0), w=[Vs])
        bias = sb([128, 14, GW], F32, "abias")
        tl = [sb([128, 4 * GW], F32, "atl") for _ in range(2)]
        PT = [sb([128, 6, GW], BF16, "aPT") for _ in range(2)]
        rz = [sb([GW, 1], F32, "arz") for _ in range(2)]
        oe = [sb([GW, R // 2, HD], BF16, "aoe") for _ in range(2)]
        it = 0
        for h in range(H):
            p.dma(qtok[:], QK[0:L, h * HD:(h + 1) * HD].rearrange("(j q) d -> q j d", q=128), r=[QK], w=[qtok])
            p.dma(ktok[:], QK[:, D + h * HD:D + (h + 1) * HD].rearrange("(j q) d -> q j d", q=128), r=[QK], w=[ktok])
            p.dma(Va[:, :, 0:HD], VV[:, h * HD:(h + 1) * HD].rearrange("(j q) d -> q j d", q=128), r=[VV], w=[Va])
            p.dma(Vs[:, :, 0:HD], VV[GW:L - GW, h * HD:(h + 1) * HD].rearrange("(j q) d -> q j d", q=128), r=[VV], w=[Vs])
            p.dma(bias[:], self.na["rpbx"][h, :, :, :], r=[self.na["rpbx"]], w=[bias])
            p.op("dve", lambda e: e.tensor_tensor(out=bias[:], in0=bias[:], in1=maskT[:].unsqueeze(1).broadcast_to([128, 14, GW]),
                                                  op=ALU.add), r=[bias, maskT], w=[bias])
            for src, dst, n in ((qtok, qT, NTQ), (ktok, kT, NTK)):
                for j0 in range(0, n, 8):
                    jn = min(8, n - j0)
                    pb = self.next_psb()
                    for j in range(j0, j0 + jn):
                        p.op("pe", lambda e: e.transpose(out=pb[:, (j - j0) * 128:(j - j0 + 1) * 128], in_=src[:, j, :],
                                                         identity=self.ident_b[:]), r=[src, self.ident_b], w=[pb])
                    p.op("act", lambda e: e.copy(out=dst[:, j0 * 128:(j0 + jn) * 128], in_=pb[:, 0:jn * 128]), r=[pb], w=[dst])
            for r in range(R):
                r0 = min(max(r - KH // 2, 0), R - KH)
                a0 = 7 - (r - r0)
                i = it % 2
                it += 1
                ps = self.next_psf()
                for kc in range(4):
                    t0 = (r0 + 2 * kc) * GW
                    p.op("pe", lambda e: e.matmul(ps[:, kc * GW:(kc + 1) * GW], lhsT=kT[:, t0:t0 + 128], rhs=qT[:, r * GW:(r + 1) * GW],
                                                  start=True, stop=True), r=[kT, qT], w=[ps])
                for cx in range(NCX):
                    p.op("pe", lambda e: e.matmul(ps[:, (4 + cx) * GW:(5 + cx) * GW], lhsT=kT[:, L + cx * 128:L + (cx + 1) * 128],
                                                  rhs=qT[:, r * GW:(r + 1) * GW], start=True, stop=True), r=[kT, qT], w=[ps])
                t_, P_ = tl[i], PT[i]
                p.op("dve", lambda e: e.tensor_tensor(out=t_[:].rearrange("q (c k) -> q c k", k=GW),
                                                      in0=ps[:, 0:4 * GW].rearrange("q (c k) -> q c k", k=GW),
                                                      in1=bias[:, a0:a0 + 7:2, :], op=ALU.add), r=[ps, bias], w=[t_])
                p.op("act", lambda e: e.activation(out=P_[:, 0:4, :].rearrange("q c k -> q (c k)"), in_=t_[:], func=AF.Exp),
                     r=[t_], w=[P_])
                p.op("act", lambda e: e.activation(out=P_[:, 4:4 + NCX, :].rearrange("q c k -> q (c k)"),
                                                   in_=ps[:, 4 * GW:(4 + NCX) * GW], func=AF.Exp), r=[ps], w=[P_])
                po = self.next_psf()
                nmm = 4 + NCX
                for kc in range(4):
                    if r0 % 2 == 0:
                        V_ = Va[:, r0 // 2 + kc, :]
                    else:
                        V_ = Vs[:, (r0 - 1) // 2 + kc, :]
                    p.op("pe", lambda e: e.matmul(po[0:GW, 0:HD + 1], lhsT=P_[:, kc, :], rhs=V_, start=(kc == 0), stop=False),
                         r=[P_, Va, Vs], w=[po])
                for cx in range(NCX):
                    p.op("pe", lambda e: e.matmul(po[0:GW, 0:HD + 1], lhsT=P_[:, 4 + cx, :], rhs=Va[:, NTQ + cx, :], start=False,
                                                  stop=(cx == NCX - 1)), r=[P_, Va], w=[po])
                z_ = rz[i]
                p.op("dve", lambda e: e.reciprocal(out=z_[:], in_=po[0:GW, HD:HD + 1]), r=[po], w=[z_])
                o_ = oe[r % 2]
                p.op("act", lambda e: e.activation(out=o_[:, r // 2, :], in_=po[0:GW, 0:HD], func=AF.Copy, scale=z_[:, 0:1]),
                     r=[po, z_], w=[o_])
            for par in range(2):
                p.dma(OO[:, h * HD:(h + 1) * HD].rearrange("(i two q) d -> two q i d", two=2, q=GW)[par], oe[par][:],
                      r=[oe[par]], w=[OO])
        p.barrier()


Builder.phase_na_attn = phase_na_attn


def phase_transpose(self, SRC, tiles, XT):
    cfg, p = self.cfg, self.p
    D, KC = cfg["D"], self.KC
    with ExitStack() as ph:
        yb = [p.sb([128, D], BF16, "ty", es=ph) for _ in range(2)]
        xT = [p.sb([128, KC, 128], BF16, "txT", es=ph) for _ in range(2)]
        for it, j in enumerate(tiles):
            y_, xT_ = yb[it % 2], xT[it % 2]
            p.dma(y_[:], SRC[j * 128:(j + 1) * 128, :], r=[SRC], w=[y_])
            for k0 in range(0, KC, 8):
                pb = self.next_psb()
                kn = min(8, KC - k0)
                for k in range(k0, k0 + kn):
                    p.op("pe", lambda e: e.transpose(out=pb[:, (k - k0) * 128:(k - k0 + 1) * 128], in_=y_[:, k * 128:(k + 1) * 128],
                                                     identity=self.ident_b[:]), r=[y_, self.ident_b], w=[pb])
                p.op("act", lambda e: e.copy(out=xT_[:, k0:k0 + kn, :].rearrange("q k t -> q (k t)"), in_=pb[:, 0:kn * 128]),
                     r=[pb], w=[xT_])
            p.dma(XT[j, :, :, :], xT_[:], r=[xT_], w=[XT])
        p.barrier()


Builder.phase_transpose = phase_transpose


def na_consts(cfg, rpb):
    GW, WW = cfg["GW"], cfg["WIN_W"]
    cols = np.arange(GW)
    c_start = np.clip(cols - WW // 2, 0, GW - WW)
    col_in = (cols[None, :] >= c_start[:, None]) & (cols[None, :] < c_start[:, None] + WW)
    col_idx = np.clip(cols[None, :] - cols[:, None], -(WW - 1), WW - 1) + (WW - 1)
    H = rpb.shape[0]
    rc = rpb[:, :, col_idx]
    rcT = rc.transpose(0, 1, 3, 2)
    out = np.zeros((H, 128, 14, GW), np.float32)
    for a in range(14):
        out[:, 0:GW, a, :] = rcT[:, a]
        out[:, GW:2 * GW, a, :] = rcT[:, a + 1]
    m = np.where(col_in.T, 0.0, -30000.0).astype(np.float32)
    maskT = np.concatenate([m, m], axis=0)
    return out, maskT
```

```python
import math
from contextlib import ExitStack
import numpy as np
import ml_dtypes
import concourse.bass as bass
import concourse.mybir as mybir
from concourse.bass_utils import run_bass_kernel_spmd

F32 = mybir.dt.float32
BF16 = mybir.dt.bfloat16
AF = mybir.ActivationFunctionType
ALU = mybir.AluOpType
AX = mybir.AxisListType
NCORES = 8

CFG_FULL = dict(D=4096, B=4, L=2048, CTX=256, GW=64, WIN_H=8, WIN_W=16, HD=128,
                PH=8, PK=128, PDK=256, TOPK=16, EMB=33, FILT=64, DEPTH=2)


class Buf:
    def __init__(self, h, name):
        self.h = h
        self.name = name
        self.lw = None
        self.rd = {}

    def __getitem__(self, idx):
        return self.h[idx]

    def ap(self):
        return self.h.ap() if hasattr(self.h, "ap") else self.h[:]


class Prog:
    NDMA = {"sp": 16, "pool": 2, "act": 2}

    def __init__(self, nc, es):
        self.nc = nc
        self.es = es
        self.engs = {"pe": nc.tensor, "act": nc.scalar, "dve": nc.vector, "pool": nc.gpsimd, "sp": nc.sync}
        self.sems = {}
        self.cnt = {}
        for e in ("pe", "act", "dve", "pool"):
            self.sems[e] = es.enter_context(nc.semaphore("s_" + e))
            self.cnt[e] = 0
        self.dma_i = {}
        for q in ("sp", "pool", "act"):
            self.dma_i[q] = 0
            for i in range(self.NDMA[q]):
                k = ("dma", q, i)
                self.sems[k] = es.enter_context(nc.semaphore("s_dma_%s_%d" % (q, i)))
                self.cnt[k] = 0
        self.sems["cc"] = es.enter_context(nc.semaphore("s_cc"))
        self.cnt["cc"] = 0
        self.waited = {e: {} for e in self.engs}
        self.nbuf = 0
        self.ninst = 0

    def sb(self, shape, dt, name=None, es=None):
        self.nbuf += 1
        name = (name or "sb") + "_%d" % self.nbuf
        b = Buf((es or self.es).enter_context(self.nc.sbuf_tensor(name, list(shape), dt)), name)
        nbytes = int(np.prod(shape[1:])) * (2 if dt == BF16 else 4)
        rem = nbytes % 64
        if rem:
            (es or self.es).enter_context(self.nc.sbuf_tensor(name + "_pad", [shape[0], 64 - rem], mybir.dt.uint8))
        return b

    def barrier(self):
        for e in self.engs:
            for k, v in self.cnt.items():
                if v > 0:
                    self._wait(e, k, v)

    def ps(self, shape, dt, name=None):
        self.nbuf += 1
        name = (name or "ps") + "_%d" % self.nbuf
        return Buf(self.es.enter_context(self.nc.psum_tensor(name, list(shape), dt)), name)

    def dram(self, shape, dt, name=None, kind=None):
        self.nbuf += 1
        name = name or ("dr_%d" % self.nbuf)
        if kind:
            return Buf(self.nc.dram_tensor(name, list(shape), dt, kind=kind), name)
        return Buf(self.nc.dram_tensor(name, list(shape), dt), name)

    def _wait(self, eng, key, val):
        w = self.waited[eng]
        if w.get(key, 0) >= val:
            return
        w[key] = val
        self.engs[eng].wait_ge(self.sems[key], val)

    def _deps(self, eng, r, w):
        deps = {}

        def add(k, v):
            if deps.get(k, 0) < v:
                deps[k] = v
        for b in r:
            if b.lw:
                add(*b.lw)
        for b in w:
            if b.lw:
                add(*b.lw)
            for k, v in b.rd.items():
                add(k, v)
        for k, v in deps.items():
            if eng == "pe" and k == "pe":
                continue
            self._wait(eng, k, v)

    def _done(self, ev, r, w):
        for b in w:
            b.lw = ev
            b.rd = {}
        for b in r:
            if b in w:
                continue
            if b.rd.get(ev[0], 0) < ev[1]:
                b.rd[ev[0]] = ev[1]

    def op(self, eng, fn, r=(), w=()):
        self._deps(eng, r, w)
        ins = fn(self.engs[eng])
        self.cnt[eng] += 1
        ins.then_inc(self.sems[eng], 1)
        self._done((eng, self.cnt[eng]), r, w)
        self.ninst += 1
        return ins

    def dma(self, out, in_, r=(), w=(), q="sp"):
        self._deps(q, r, w)
        i = self.dma_i[q]
        self.dma_i[q] += 1
        k = ("dma", q, i % self.NDMA[q])
        self._wait(q, k, self.cnt[k])
        ins = self.engs[q].dma_start(out=out, in_=in_)
        self.cnt[k] += 16
        ins.then_inc(self.sems[k], 16)
        self._done((k, self.cnt[k]), r, w)
        self.ninst += 1

    def allgather(self, out_b, in_b):
        self._deps("pool", [in_b], [out_b])
        ins = self.nc.gpsimd.collective_compute("AllGather", ALU.bypass, replica_groups=[list(range(NCORES))],
                                                ins=[in_b.ap()], outs=[out_b.ap()])
        self.cnt["cc"] += 1
        ins.then_inc(self.sems["cc"])
        self._done(("cc", self.cnt["cc"]), [in_b], [out_b])

    def finish(self, bufs):
        for b in bufs:
            if b.lw:
                self._wait("sp", *b.lw)


def _cdiv(a, b):
    return (a + b - 1) // b


class Builder:
    def __init__(self, cfg, debug=()):
        self.cfg = cfg
        self.debug = set(debug)
        self.inputs = {}

    def build(self):
        cfg = self.cfg
        D, L, CTX = cfg["D"], cfg["L"], cfg["CTX"]
        T = L + CTX
        KC = D // 128
        nc = bass.Bass("TRN2", target_bir_lowering=False)
        self.nc = nc
        es = ExitStack()
        self.es = es
        p = Prog(nc, es)
        self.p = p
        self.T, self.KC = T, KC

        def inp(name, shape, dt=F32):
            b = p.dram(shape, dt, name=name, kind="ExternalInput")
            self.inputs[name] = (tuple(shape), dt)
            return b
        self.inp = inp
        self.outs = {}

        def outp(name, shape, dt=F32):
            b = p.dram(shape, dt, name=name, kind="ExternalOutput")
            self.outs[name] = b
            return b
        self.outp = outp

        self.psf = [p.ps([128, 512], F32, "psf") for _ in range(5)]
        self.psx = p.ps([128, 512], F32, "psx")
        self.psb = [p.ps([128, 1024], BF16, "psb") for _ in range(2)]
        self.psf_i = 0
        self.psb_i = 0

        ident_b_d = inp("ident_b", [128, 128], BF16)
        ident_f_d = inp("ident_f", [128, 128], F32)
        self.ident_b = p.sb([128, 128], BF16, "identb")
        self.ident_f = p.sb([128, 128], F32, "identf")
        p.dma(self.ident_b[:], ident_b_d[:, :], r=[ident_b_d], w=[self.ident_b])
        p.dma(self.ident_f[:], ident_f_d[:, :], r=[ident_f_d], w=[self.ident_f])
        self.ones_f = p.sb([128, 128], F32, "onesf")
        p.op("dve", lambda e: e.memset(self.ones_f[:], 1.0), w=[self.ones_f])
        self.eps_t = p.sb([128, 1], F32, "eps")
        p.op("dve", lambda e: e.memset(self.eps_t[:], 1e-6), w=[self.eps_t])

        self.HX = p.dram([T, D], F32, "HX")
        x_in = inp("x", [L, D])
        ctx_in = inp("ctx", [CTX, D])
        p.dma(self.HX[0:L, :], x_in[:, :], r=[x_in], w=[self.HX])
        p.dma(self.HX[L:T, :], ctx_in[:, :], r=[ctx_in], w=[self.HX])

        self.phase_weights()
        self.phase_ada()
        for l in range(cfg["DEPTH"]):
            if "stopw" in self.debug:
                break
            self.layer(l)
            if "only0" in self.debug:
                break

        out = outp("out", [L, D])
        p.dma(out[:, :], self.HX[0:L, :], r=[self.HX], w=[out])
        p.finish(list(self.outs.values()))
        es.close()
        return nc

    def next_psf(self):
        b = self.psf[self.psf_i % len(self.psf)]
        self.psf_i += 1
        return b

    def next_psb(self):
        b = self.psb[self.psb_i % len(self.psb)]
        self.psb_i += 1
        return b

    def gather_weight(self, name, K, N, src_dt=F32):
        p = self.p
        Ks = K // NCORES
        src = self.inp(name, [Ks, N], src_dt)
        full = p.dram([K, N], BF16, name + "_full")
        if src_dt == BF16:
            bounce = p.dram([Ks, N], BF16, name + "_bn")
            p.dma(bounce[:, :], src[:, :], r=[src], w=[bounce])
            p.allgather(full, bounce)
            return full
        bounce = p.dram([Ks, N], BF16, name + "_bn")
        RT = min(128, Ks)
        CT = min(N, 4096)
        engs = ["dve", "pool", "act"]
        for r0 in range(0, Ks, RT):
            for c0 in range(0, N, CT):
                i = self.wi
                self.wi += 1
                st, sb_ = self.wst[i % 2], self.wsb[i % 2]
                p.dma(st[0:RT, 0:CT], src[r0:r0 + RT, c0:c0 + CT], r=[src], w=[st], q="sp")
                e = engs[i % 3]
                if e == "act":
                    p.op("act", lambda E: E.copy(out=sb_[0:RT, 0:CT], in_=st[0:RT, 0:CT]), r=[st], w=[sb_])
                else:
                    p.op(e, lambda E: E.tensor_copy(out=sb_[0:RT, 0:CT], in_=st[0:RT, 0:CT]), r=[st], w=[sb_])
                p.dma(bounce[r0:r0 + RT, c0:c0 + CT], sb_[0:RT, 0:CT], r=[sb_], w=[bounce], q="sp")
        p.allgather(full, bounce)
        return full

    def phase_weights(self):
        cfg, p = self.cfg, self.p
        D, L, CTX = cfg["D"], cfg["L"], cfg["CTX"]
        E = cfg["PK"] ** 2
        NQ = cfg["PH"] * cfg["PDK"]
        self.wi = 0
        with ExitStack() as ph:
            self.wst = [p.sb([128, 4096], F32, "wst", es=ph) for _ in range(2)]
            self.wsb = [p.sb([128, 4096], BF16, "wsb", es=ph) for _ in range(2)]
            W = {}
            W["hy_w_in"] = self.gather_weight("hy_w_in", D, 3 * D)
            W["hy_w_out"] = self.gather_weight("hy_w_out", D, D)
            W["na_w_qkv"] = self.gather_weight("na_w_qkv", D, 3 * D)
            W["na_w_out"] = self.gather_weight("na_w_out", D, D)
            for l in range(cfg["DEPTH"]):
                W["peer_wq%d" % l] = self.gather_weight("peer_wq%d" % l, NQ, D)
                W["peer_uT%d" % l] = self.gather_weight("peer_uT%d" % l, E, D)
                W["peer_v%d" % l] = self.gather_weight("peer_v%d" % l, E, D)
            W["wf_l"] = self.gather_weight("wf_l", 2 * L, L, BF16)
            W["wi_l"] = self.gather_weight("wi_l", L, 2 * L, BF16)
            W["wf_c"] = self.gather_weight("wf_c", 2 * CTX, CTX, BF16)
            W["wi_c"] = self.gather_weight("wi_c", CTX, 2 * CTX, BF16)
            self.W = W
            p.barrier()

    def phase_ada(self):
        cfg, p = self.cfg, self.p
        D, KC, DEPTH = cfg["D"], self.KC, cfg["DEPTH"]
        NL = 6 * D // NCORES
        c_all = self.inp("c_all", [5, D])
        ada_w = self.inp("ada_w", [DEPTH, D, NL])
        ada_b = self.inp("ada_b", [DEPTH, NL])
        self.sel_x_d = self.inp("sel_x", [5, 128])
        self.sel_c_d = self.inp("sel_c", [5, 128])
        self.sel_x = p.sb([5, 128], F32, "selx")
        self.sel_c = p.sb([5, 128], F32, "selc")
        p.dma(self.sel_x[:], self.sel_x_d[:, :], r=[self.sel_x_d], w=[self.sel_x])
        p.dma(self.sel_c[:], self.sel_c_d[:, :], r=[self.sel_c_d], w=[self.sel_c])
        mod_loc = p.dram([DEPTH, 5, NL], F32, "mod_loc")
        self.MOD = p.dram([NCORES, DEPTH, 5, 6, D // NCORES], F32, "MOD")
        NT = _cdiv(NL, 512)
        with ExitStack() as ph:
            cs = p.sb([5, D], F32, "cs", es=ph)
            p.dma(cs[:], c_all[:, :], r=[c_all], w=[cs])
            p.op("act", lambda e: e.activation(out=cs[:], in_=cs[:], func=AF.Silu), r=[cs], w=[cs])
            sT = p.sb([128, KC, 5], F32, "sT", es=ph)
            pst = self.next_psf()
            for k in range(KC):
                p.op("pe", lambda e: e.transpose(out=pst[:, k * 5:(k + 1) * 5], in_=cs[0:5, k * 128:(k + 1) * 128],
                                                 identity=self.ident_f[0:5, 0:5]), r=[cs, self.ident_f], w=[pst])
            p.op("dve", lambda e: e.tensor_copy(out=sT[:].rearrange("p k m -> p (k m)"), in_=pst[:, 0:KC * 5]),
                 r=[pst], w=[sT])
            wt = [p.sb([128, NL], F32, "adaw", es=ph) for _ in range(2)]
            bt = p.sb([5, NL], F32, "adab", es=ph)
            res = p.sb([5, NL], F32, "adares", es=ph)
            for l in range(DEPTH):
                p.dma(bt[:], ada_b[l, :].partition_broadcast(5), r=[ada_b], w=[bt])
                pss = [self.next_psf() for _ in range(min(NT, 5))] + ([self.psx] if NT == 6 else [])
                for k in range(KC):
                    w = wt[k % 2]
                    p.dma(w[:], ada_w[l, k * 128:(k + 1) * 128, :], r=[ada_w], w=[w])
                    for j in range(NT):
                        n0, n1 = j * 512, min(NL, (j + 1) * 512)
                        p.op("pe", lambda e: e.matmul(pss[j][0:5, 0:n1 - n0], lhsT=sT[:, k, :], rhs=w[:, n0:n1],
                                                      start=(k == 0), stop=(k == KC - 1)), r=[sT, w], w=[pss[j]])
                for j in range(NT):
                    n0, n1 = j * 512, min(NL, (j + 1) * 512)
                    p.op("dve", lambda e: e.tensor_tensor(out=res[:, n0:n1], in0=pss[j][0:5, 0:n1 - n0], in1=bt[:, n0:n1],
                                                          op=ALU.add), r=[pss[j], bt], w=[res])
                p.dma(mod_loc[l, :, :], res[:], r=[res], w=[mod_loc])
            p.allgather(self.MOD, mod_loc)
            p.barrier()

    def mod_tile(self, dst, l, k, stream, plus_one=False):
        cfg, p = self.cfg, self.p
        D = cfg["D"]
        rows = self.modrows
        p.dma(rows[:].rearrange("m (r i) -> m r i", r=NCORES),
              self.MOD[:, l, :, k, :].rearrange("r m i -> m r i"), r=[self.MOD], w=[rows])
        sel = self.sel_x if stream == "x" else self.sel_c
        for n0 in range(0, D, 512):
            n1 = min(D, n0 + 512)
            ps = self.next_psf()
            p.op("pe", lambda e: e.matmul(ps[:, 0:n1 - n0], lhsT=sel[:], rhs=rows[:, n0:n1], start=True, stop=True),
                 r=[sel, rows], w=[ps])
            if plus_one:
                p.op("dve", lambda e: e.tensor_scalar(out=dst[:, n0:n1], in0=ps[:, 0:n1 - n0], scalar1=1.0, scalar2=None,
                                                      op0=ALU.add), r=[ps], w=[dst])
            else:
                p.op("dve", lambda e: e.tensor_copy(out=dst[:, n0:n1], in_=ps[:, 0:n1 - n0]), r=[ps], w=[dst])

    def bcast_row(self, dst, src_ap, src_buf, n):
        self.p.dma(dst[:, 0:n], src_ap.partition_broadcast(128), r=[src_buf], w=[dst])

    def phase_norm(self, l, sub, gname, tiles_x, tiles_c, XT):
        cfg, p = self.cfg, self.p
        D, KC = cfg["D"], self.KC
        g_d = self.g_in[gname]
        with ExitStack() as ph:
            self.modrows = p.sb([5, D], F32, "modrows", es=ph)
            gt = p.sb([128, D], F32, "gt", es=ph)
            self.bcast_row(gt, g_d[l, :], g_d, D)
            A1 = p.sb([128, D], F32, "modA", es=ph)
            B1 = p.sb([128, D], F32, "modB", es=ph)
            A = {"x": A1, "c": A1}
            Bt = {"x": B1, "c": B1}
            old = True
            if old:
                A["c"] = p.sb([128, D], F32, "modA", es=ph)
                Bt["c"] = p.sb([128, D], F32, "modB", es=ph)
                for stream, tiles in (("x", tiles_x), ("c", tiles_c)):
                    if tiles:
                        self.mod_tile(A[stream], l, 3 * sub + 1, stream, plus_one=True)
                        p.op("pool", lambda e: e.tensor_tensor(out=A[stream][:], in0=A[stream][:], in1=gt[:], op=ALU.mult),
                             r=[A[stream], gt], w=[A[stream]])
                        self.mod_tile(Bt[stream], l, 3 * sub, stream)
            if "dummy" in self.debug:
                _d1 = p.sb([128, D], F32, "dummy", es=ph)
                _d2 = p.sb([128, D], F32, "dummy", es=ph)
            xt = [p.sb([128, D], F32, "nx", es=ph) for _ in range(2)]
            yb = [p.sb([128, D], BF16, "ny", es=ph) for _ in range(2)]
            junk = p.sb([128, D], BF16, "njunk", es=ph)
            ss = [p.sb([128, 1], F32, "nss", es=ph) for _ in range(2)]
            xT = [p.sb([128, KC, 128], BF16, "nxT", es=ph) for _ in range(2)]
            it = 0
            for stream, tiles in (("x", tiles_x), ("c", tiles_c)):
                if not tiles:
                    continue
                if not old:
                    self.mod_tile(A1, l, 3 * sub + 1, stream, plus_one=True)
                    p.op("pool", lambda e: e.tensor_tensor(out=A1[:], in0=A1[:], in1=gt[:], op=ALU.mult), r=[A1, gt], w=[A1])
                    self.mod_tile(B1, l, 3 * sub, stream)
                for j in tiles:
                    x_, y_, s_, xT_ = xt[it % 2], yb[it % 2], ss[it % 2], xT[it % 2]
                    it += 1
                    p.dma(x_[:], self.HX[j * 128:(j + 1) * 128, :], r=[self.HX], w=[x_])
                    p.op("act", lambda e: e.activation(out=junk[:], in_=x_[:], func=AF.Square, accum_out=s_[:]),
                         r=[x_], w=[junk, s_])
                    p.op("dve", lambda e: e.tensor_scalar(out=s_[:], in0=s_[:], scalar1=1.0 / D, scalar2=1e-6,
                                                          op0=ALU.mult, op1=ALU.add), r=[s_], w=[s_])
                    p.op("act", lambda e: e.activation(out=s_[:], in_=s_[:], func=AF.Sqrt), r=[s_], w=[s_])
                    p.op("dve", lambda e: e.reciprocal(out=s_[:], in_=s_[:]), r=[s_], w=[s_])
                    p.op("dve", lambda e: e.scalar_tensor_tensor(out=x_[:], in0=x_[:], scalar=s_[:, 0:1], in1=A[stream][:],
                                                                 op0=ALU.mult, op1=ALU.mult), r=[x_, s_, A[stream]], w=[x_])
                    p.op("dve" if "nopool" in self.debug else "pool", lambda e: e.tensor_tensor(out=y_[:], in0=x_[:], in1=Bt[stream][:], op=ALU.add),
                         r=[x_, Bt[stream]], w=[y_])
                    for k0 in range(0, KC, 8):
                        pb = self.next_psb()
                        kn = min(8, KC - k0)
                        for k in range(k0, k0 + kn):
                            p.op("pe", lambda e: e.transpose(out=pb[:, (k - k0) * 128:(k - k0 + 1) * 128],
                                                             in_=y_[:, k * 128:(k + 1) * 128], identity=self.ident_b[:]),
                                 r=[y_, self.ident_b], w=[pb])
                        eng = "act" if (k0 // 8) % 2 else "dve"
                        if eng == "act":
                            p.op("act", lambda e: e.copy(out=xT_[:, k0:k0 + kn, :].rearrange("p k t -> p (k t)"),
                                                         in_=pb[:, 0:kn * 128]), r=[pb], w=[xT_])
                        else:
                            p.op("dve", lambda e: e.tensor_copy(out=xT_[:, k0:k0 + kn, :].rearrange("p k t -> p (k t)"),
                                                                in_=pb[:, 0:kn * 128]), r=[pb], w=[xT_])
                    p.dma(XT[j, :, :, :], xT_[:], r=[xT_], w=[XT])
            p.barrier()

    def layer(self, l):
        cfg, p = self.cfg, self.p
        D, L, CTX, T, KC = cfg["D"], cfg["L"], cfg["CTX"], self.T, self.KC
        last = l == cfg["DEPTH"] - 1
        if l == 0:
            self.g_in = {"norm1_g": self.inp("norm1_g", [cfg["DEPTH"], D]), "norm2_g": self.inp("norm2_g", [cfg["DEPTH"], D])}
            self.XT = p.dram([T // 128, 128, KC, 128], BF16, "XT")
        tiles_x = list(range(L // 128))
        tiles_c = list(range(L // 128, T // 128))
        self.phase_norm(l, 0, "norm1_g", tiles_x, [] if "noc" in self.debug else tiles_c, self.XT)
        if "xt1_%d" % l in self.debug:
            o = self.outp("dbg_xt1_%d" % l, [T // 128, 128, KC, 128], BF16)
            p.dma(o.ap(), self.XT.ap(), r=[self.XT], w=[o])
        if "stopn_%d" % l in self.debug:
            return
        if l == 0:
            self.ZT = p.dram([T // 128, 128, KC, 128], BF16, "ZT")
        if l % 2 == 0:
            self.phase_hy_inproj(l)
            self.phase_hy_conv(l)
            if "zt_%d" % l in self.debug:
                o = self.outp("dbg_zt_%d" % l, [T // 128, 128, KC, 128], BF16)
                p.dma(o.ap(), self.ZT.ap(), r=[self.ZT], w=[o])
            self.phase_outproj(l, self.ZT, self.W["hy_w_out"], self.hy["hy_b_out"], 2, tiles_x, tiles_c if not last else [])
        if l % 2 == 1:
            assert last
            self.phase_na_qkv(l)
            self.phase_na_attn(l)
            if "oo_%d" % l in self.debug:
                o = self.outp("dbg_oo_%d" % l, [L, D], BF16)
                p.dma(o.ap(), self.OO.ap(), r=[self.OO], w=[o])
            self.phase_transpose(self.OO, tiles_x, self.ZT)
            self.phase_outproj(l, self.ZT, self.W["na_w_out"], None, 2, tiles_x, [])
        if "hx1_%d" % l in self.debug:
            o = self.outp("dbg_hx1_%d" % l, [T, D], F32)
            p.dma(o.ap(), self.HX.ap(), r=[self.HX], w=[o])
        if "stop1_%d" % l in self.debug:
            return
        tc2 = tiles_c if not last else []
        self.phase_norm(l, 1, "norm2_g", tiles_x, tc2, self.XT)
        if "stopn2_%d" % l in self.debug:
            return
        self.phase_peer(l, tiles_x, tc2)
        if "hx2_%d" % l in self.debug:
            o = self.outp("dbg_hx2_%d" % l, [T, D], F32)
            p.dma(o.ap(), self.HX.ap(), r=[self.HX], w=[o])


def _bf(a):
    return np.ascontiguousarray(a).astype(ml_dtypes.bfloat16)


def dft_mats(L):
    N = 2 * L
    s = np.arange(L, dtype=np.float64)[:, None]
    f = np.arange(L, dtype=np.float64)[None, :]
    ang = 2 * np.pi * f * s / N
    WF = np.zeros((L, N))
    WF[:, :L] = np.cos(ang)
    WF[:, L] = (-1.0) ** np.arange(L)
    WF[:, L + 1:] = -np.sin(ang[:, 1:])
    WI = np.zeros((N, L))
    WI[:L, :] = (2.0 / N) * np.cos(ang.T)
    WI[0, :] = 1.0 / N
    WI[L, :] = (1.0 / N) * (-1.0) ** np.arange(L)
    WI[L + 1:, :] = -(2.0 / N) * np.sin(ang.T[1:, :])
    return WF, WI


def host_inputs(cfg, inputs, core, names):
    D, L, CTX = cfg["D"], cfg["L"], cfg["CTX"]
    b = core % cfg["B"]
    sh = lambda a: np.ascontiguousarray(np.array_split(a, NCORES, axis=0)[core])
    f32 = lambda a: np.ascontiguousarray(np.asarray(a, dtype=np.float32))
    m = {}
    C = _CONST_CACHE.setdefault((L, CTX), {})
    if not C:
        for nm, Lx in (("l", L), ("c", CTX)):
            WF, WI = dft_mats(Lx)
            n = Lx // 128
            C["wf_" + nm] = _bf(WF.reshape(n, 128, 2 * n, 128).transpose(2, 1, 0, 3).reshape(2 * Lx, Lx))
            C["wi_" + nm] = _bf(WI.reshape(2 * n, 128, n, 128).transpose(2, 1, 0, 3).reshape(Lx, 2 * Lx))
        C.update(hyena_consts(cfg))
    m["ident_b"] = _bf(np.eye(128))
    m["ident_f"] = np.eye(128, dtype=np.float32)
    m["x"] = f32(inputs["x"][b])
    m["ctx"] = f32(inputs["ctx"][b])
    for k in ("hy_w_in", "hy_w_out", "na_w_qkv", "na_w_out"):
        m[k] = sh(f32(inputs[k][0]))
    for l in range(cfg["DEPTH"]):
        KCh = D // 128
        wq = f32(inputs["peer_wq"][l])
        NQ = wq.shape[1]
        m["peer_wq%d" % l] = sh(wq.reshape(KCh, 128, NQ // 128, 128).transpose(2, 1, 0, 3).reshape(NQ, D))
        u = f32(inputs["peer_u"][l])
        m["peer_uT%d" % l] = sh(u.reshape(u.shape[0] // 128, 128, KCh, 128).transpose(0, 3, 2, 1).reshape(u.shape[0], D))
        m["peer_k1T%d" % l] = np.ascontiguousarray(f32(inputs["peer_k1"][l]).transpose(2, 0, 1))
        m["peer_k2T%d" % l] = np.ascontiguousarray(f32(inputs["peer_k2"][l]).transpose(2, 0, 1))
        m["peer_v%d" % l] = sh(f32(inputs["peer_v"][l]))
    for k in ("wf_l", "wi_l", "wf_c", "wi_c"):
        m[k] = sh(C[k])
    m["c_all"] = f32(np.concatenate([inputs["c"], inputs["c_ctx"][None]], axis=0))
    aw = f32(inputs["ada_w"]).reshape(cfg["DEPTH"], D, 6, NCORES, D // NCORES)
    m["ada_w"] = np.ascontiguousarray(aw[:, :, :, core, :]).reshape(cfg["DEPTH"], D, 6 * D // NCORES)
    ab = f32(inputs["ada_b"]).reshape(cfg["DEPTH"], 6, NCORES, D // NCORES)
    m["ada_b"] = np.ascontiguousarray(ab[:, :, core, :]).reshape(cfg["DEPTH"], 6 * D // NCORES)
    sx = np.zeros((5, 128), np.float32)
    sx[b] = 1.0
    sc = np.zeros((5, 128), np.float32)
    sc[4] = 1.0
    m["sel_x"], m["sel_c"] = sx, sc
    m["norm1_g"], m["norm2_g"] = f32(inputs["norm1_g"]), f32(inputs["norm2_g"])
    for k in ("hy_b_in", "hy_conv_w", "hy_conv_b", "hy_skip", "hy_b_out", "hy_f_w1", "hy_f_w2", "hy_f_w3", "hy_f_w4"):
        m[k] = f32(inputs[k][0])
    for k in ("hy_f_b1", "hy_f_b2", "hy_f_b3", "hy_f_freq"):
        m[k] = f32(inputs[k][0]).reshape(-1, 1)
    for k in ("zT_l", "zT_c", "negt_l", "negt_c", "deltas"):
        m[k] = C[k]
    m["na_q_g"], m["na_k_g"] = f32(inputs["na_q_g"][0]), f32(inputs["na_k_g"][0])
    m["na_rpbx"], m["na_maskT"] = na_consts(cfg, f32(inputs["na_rpb"][0]))
    return {k: m[k] for k in names}


_CONST_CACHE = {}


def run(cfg, inputs, debug=()):
    bld = Builder(cfg, debug)
    nc = bld.build()
    names = list(bld.inputs.keys())
    in_maps = [host_inputs(cfg, inputs, c, names) for c in range(NCORES)]
    for m in in_maps:
        for k, (shape, dt) in bld.inputs.items():
            assert tuple(m[k].shape) == tuple(shape), (k, m[k].shape, shape)
    res = run_bass_kernel_spmd(nc, in_maps, core_ids=list(range(NCORES)))
    return res, bld


def kernel(**inputs):
    cfg = CFG_FULL
    res, bld = run(cfg, inputs)
    out = np.stack([np.asarray(res.results[b]["out"], dtype=np.float32) for b in range(cfg["B"])], axis=0)
    return out


def phase_proj(self, XT, tiles, Wfull, N, evac, ncol=512):
    cfg, p = self.cfg, self.p
    KC = self.KC
    with ExitStack() as ph:
        wt = [p.sb([128, KC, ncol], BF16, "pw", es=ph) for _ in range(2)]
        xt = [p.sb([128, KC, 128], BF16, "px", es=ph) for _ in range(3)]
        self.proj_ph = ph
        it = 0
        Wv = Wfull.ap().rearrange("(k p) n -> p k n", p=128)
        for ci, c0 in enumerate(range(0, N, ncol)):
            c1 = min(N, c0 + ncol)
            w = wt[ci % 2]
            p.dma(w[:, :, 0:c1 - c0], Wv[:, :, c0:c1], r=[Wfull], w=[w])
            for j in tiles:
                x_ = xt[it % 3]
                it += 1
                p.dma(x_[:], XT[j, :, :, :], r=[XT], w=[x_])
                ps = self.next_psf()
                for k in range(KC):
                    p.op("pe", lambda e: e.matmul(ps[:, 0:c1 - c0], lhsT=x_[:, k, :], rhs=w[:, k, 0:c1 - c0],
                                                  start=(k == 0), stop=(k == KC - 1)), r=[x_, w], w=[ps])
                evac(ps, j, c0, c1)
        p.barrier()


Builder.phase_proj = phase_proj


def phase_hy_inproj(self, l):
    cfg, p = self.cfg, self.p
    D, L, CTX, T = cfg["D"], cfg["L"], cfg["CTX"], self.T
    N = 3 * D
    if not hasattr(self, "PP"):
        self.PP = p.dram([T + 4, N], F32, "PP")
        self.hy = {k: self.inp(k, shp) for k, shp in [
            ("hy_b_in", [N]), ("hy_conv_w", [3, N]), ("hy_conv_b", [N]), ("hy_skip", [2, D]), ("hy_b_out", [D]),
            ("hy_f_w1", [cfg["EMB"], cfg["FILT"]]), ("hy_f_b1", [cfg["FILT"], 1]), ("hy_f_w2", [cfg["FILT"], cfg["FILT"]]),
            ("hy_f_b2", [cfg["FILT"], 1]), ("hy_f_w3", [cfg["FILT"], cfg["FILT"]]), ("hy_f_b3", [cfg["FILT"], 1]),
            ("hy_f_w4", [cfg["FILT"], 4 * D]), ("hy_f_freq", [cfg["FILT"], 1]),
            ("zT_l", [cfg["EMB"], L]), ("zT_c", [cfg["EMB"], CTX]), ("negt_l", [128, L // 128]),
            ("negt_c", [128, CTX // 128]), ("deltas", [D])]}
    PP = self.PP
    with ExitStack() as ph:
        zt = p.sb([128, 512], F32, "zero", es=ph)
        p.op("dve", lambda e: e.memset(zt[:], 0.0), w=[zt])
        for r in (0, L + 1, L + 2, L + 3 + CTX):
            for c0 in range(0, N, 512 * 128):
                n = min(N - c0, 512 * 128)
                p.dma(PP[r, c0:c0 + n].rearrange("(a b) -> a b", b=512), zt[0:n // 512, :], r=[zt], w=[PP])
        bias = p.sb([128, N], F32, "hbin", es=ph)
        self.bcast_row(bias, self.hy["hy_b_in"][:], self.hy["hy_b_in"], N)
        ot = [p.sb([128, 512], F32, "po", es=ph) for _ in range(3)]
        cnt = [0]

        def evac(ps, j, c0, c1):
            o = ot[cnt[0] % 3]
            cnt[0] += 1
            p.op("dve", lambda e: e.tensor_tensor(out=o[:, 0:c1 - c0], in0=ps[:, 0:c1 - c0], in1=bias[:, c0:c1], op=ALU.add),
                 r=[ps, bias], w=[o])
            r0 = 1 + j * 128 if j < L // 128 else L + 3 + (j - L // 128) * 128
            p.dma(PP[r0:r0 + 128, c0:c1], o[:, 0:c1 - c0], r=[o], w=[PP])
        self.phase_proj(self.XT, list(range(T // 128)), self.W["hy_w_in"], N, evac)


Builder.phase_hy_inproj = phase_hy_inproj


def phase_hy_conv(self, l):
    cfg, p = self.cfg, self.p
    D, L, CTX, T, KC = cfg["D"], cfg["L"], cfg["CTX"], self.T, self.KC
    FI = cfg["FILT"]
    CT = min(256, D)
    NL, NC_ = L // 128, CTX // 128
    NCH = NL + NC_
    hy, PP = self.hy, self.PP
    segs = [(0, NL, self.W["wf_l"], self.W["wi_l"], 0, "l"), (NL, NC_, self.W["wf_c"], self.W["wi_c"], 2 * NL, "c")]
    PI = math.pi
    with ExitStack() as ph:
        sb = lambda shape, dt, name: p.sb(shape, dt, name, es=ph)
        h3T = sb([FI, T], F32, "h3T")
        ph_outer = ph
        ph = ExitStack()
        sb = lambda shape, dt, name: p.sb(shape, dt, name, es=ph)
        w1 = sb([cfg["EMB"], FI], F32, "fw1")
        w2 = sb([FI, FI], F32, "fw2")
        w3 = sb([FI, FI], F32, "fw3")
        fb = [sb([FI, 1], F32, "fb") for _ in range(3)]
        fr = sb([FI, 1], F32, "ffr")
        p.dma(w1[:], hy["hy_f_w1"][:, :], r=[hy["hy_f_w1"]], w=[w1])
        p.dma(w2[:], hy["hy_f_w2"][:, :], r=[hy["hy_f_w2"]], w=[w2])
        p.dma(w3[:], hy["hy_f_w3"][:, :], r=[hy["hy_f_w3"]], w=[w3])
        for i, k in enumerate(("hy_f_b1", "hy_f_b2", "hy_f_b3")):
            p.dma(fb[i][:], hy[k][:, :], r=[hy[k]], w=[fb[i]])
        p.dma(fr[:], hy["hy_f_freq"][:, :], r=[hy["hy_f_freq"]], w=[fr])
        zT = sb([cfg["EMB"], T], F32, "zT")
        p.dma(zT[:, 0:L], hy["zT_l"][:, :], r=[hy["zT_l"]], w=[zT])
        p.dma(zT[:, L:T], hy["zT_c"][:, :], r=[hy["zT_c"]], w=[zT])
        hA = sb([FI, T], F32, "hA")
        hB = sb([FI, T], F32, "hB")
        arg = sb([FI, 512], F32, "farg")
        argm = sb([FI, 512], F32, "fargm")
        for li, (wl, src, dst) in enumerate(((w1, zT, hA), (w2, hA, hB), (w3, hB, h3T))):
            for n0 in range(0, T, 512):
                n1 = min(T, n0 + 512)
                ps = self.next_psf()
                p.op("pe", lambda e: e.matmul(ps[0:FI, 0:n1 - n0], lhsT=wl[:], rhs=src[:, n0:n1], start=True, stop=True),
                     r=[wl, src], w=[ps])
                p.op("dve", lambda e: e.tensor_scalar(out=arg[:, 0:n1 - n0], in0=ps[0:FI, 0:n1 - n0], scalar1=fb[li][:, 0:1],
                                                      scalar2=fr[:, 0:1], op0=ALU.add, op1=ALU.mult), r=[ps, fb[li], fr], w=[arg])
                for _ in range(2):
                    for thr, cmp, add in ((PI, ALU.is_gt, -2 * PI), (-PI, ALU.is_lt, 2 * PI)):
                        p.op("dve", lambda e: e.tensor_scalar(out=argm[:, 0:n1 - n0], in0=arg[:, 0:n1 - n0], scalar1=thr, scalar2=add,
                                                              op0=cmp, op1=ALU.mult), r=[arg], w=[argm])
                        p.op("dve", lambda e: e.tensor_tensor(out=arg[:, 0:n1 - n0], in0=arg[:, 0:n1 - n0], in1=argm[:, 0:n1 - n0],
                                                              op=ALU.add), r=[arg, argm], w=[arg])
                p.op("act", lambda e: e.activation(out=dst[:, n0:n1], in_=arg[:, 0:n1 - n0], func=AF.Sin), r=[arg], w=[dst])
        p.barrier()
        ph.close()
        ph = ph_outer
        sb = lambda shape, dt, name: p.sb(shape, dt, name, es=ph)
        negt = sb([128, NCH], F32, "negt")
        p.dma(negt[:, 0:NL], hy["negt_l"][:, :], r=[hy["negt_l"]], w=[negt])
        p.dma(negt[:, NL:NCH], hy["negt_c"][:, :], r=[hy["negt_c"]], w=[negt])
        tmp = [sb([128, NCH, CT], F32, "ctmp")] * 2
        V = sb([128, NCH, CT], F32, "cV")
        X = sb([128, NCH, CT], F32, "cX")
        Ub = sb([128, NCH, CT], BF16, "cUb")
        Y = sb([128, 2 * NCH, CT], BF16, "cY")
        gp = sb([128, NCH, CT], BF16, "cgp")
        gm = sb([128, NCH, CT], BF16, "cgm")
        KA = sb([128, NCH, CT], BF16, "cKA")
        KB = sb([128, NCH, CT], BF16, "cKB")
        KD0 = [sb([128, CT], BF16, "cKD0") for _ in range(2)]
        wfs = [sb([128, NL, 128], BF16, "wfs") for _ in range(2)]
        wis = [sb([128, 2 * NL, 128], BF16, "wis") for _ in range(2)]
        rows = {k: sb([128, CT], F32, "crow_" + k) for k in
                ["w0", "w1", "w2", "cb", "sk0", "sk1", "dl"]}
        w4t = sb([FI, 4, CT], F32, "w4t")
        wn = [sb([128, CT], F32, "cwn") for _ in range(2)]
        sm = [sb([128, CT], F32, "csm") for _ in range(6)]
        rns = [sb([128, CT], F32, "crn") for _ in range(2)]
        zT_ = sb([128, CT // 128, T], BF16, "czT")
        self.slab_i = 0
        self.sm_i = 0
        ZTv = self.ZT.ap().rearrange("j p k t -> p k j t")

        def bc(t):
            return t[:].unsqueeze(1).broadcast_to([128, NCH, CT])

        def short_conv(g, c0, dst):
            col = g * D + c0
            for k, src in (("w0", hy["hy_conv_w"][0, col:col + CT]), ("w1", hy["hy_conv_w"][1, col:col + CT]),
                           ("w2", hy["hy_conv_w"][2, col:col + CT]), ("cb", hy["hy_conv_b"][col:col + CT])):
                p.dma(rows[k][:], src.partition_broadcast(128), r=[hy["hy_conv_w"], hy["hy_conv_b"]], w=[rows[k]])

            def load(tap, t_):
                p.dma(t_[:, 0:NL, :], PP[tap:tap + L, col:col + CT].rearrange("(j q) c -> q j c", q=128), r=[PP], w=[t_])
                p.dma(t_[:, NL:NCH, :], PP[L + 2 + tap:L + 2 + tap + CTX, col:col + CT].rearrange("(j q) c -> q j c", q=128),
                      r=[PP], w=[t_])
            load(0, tmp[0])
            p.op("pool", lambda e: e.tensor_tensor(out=dst[:], in0=tmp[0][:], in1=bc(rows["w0"]), op=ALU.mult),
                 r=[tmp[0], rows["w0"]], w=[dst])
            p.op("dve", lambda e: e.tensor_tensor(out=dst[:], in0=dst[:], in1=bc(rows["cb"]), op=ALU.add),
                 r=[dst, rows["cb"]], w=[dst])
            for tap in (1, 2):
                t_ = tmp[tap % 2]
                load(tap, t_)
                p.op("pool", lambda e: e.tensor_tensor(out=t_[:], in0=t_[:], in1=bc(rows["w%d" % tap]), op=ALU.mult),
                     r=[t_, rows["w%d" % tap]], w=[t_])
                p.op("dve", lambda e: e.tensor_tensor(out=dst[:], in0=dst[:], in1=t_[:], op=ALU.add), r=[dst, t_], w=[dst])

        def fwd(U, seg, parts, cb):
            off, n, WF, WI, yoff, nm = seg
            for kind, j in parts:
                ic = j if kind == "re" else n + j
                slab = wfs[self.slab_i % 2]
                self.slab_i += 1
                p.dma(slab[:, 0:n, :], WF[ic * 128:(ic + 1) * 128, :].rearrange("q (s i) -> q s i", i=128), r=[WF], w=[slab])
                ps = self.next_psf()
                for s in range(n):
                    p.op("pe", lambda e: e.matmul(ps[:, 0:CT], lhsT=slab[:, s, :], rhs=U[:, off + s, :], start=(s == 0),
                                                  stop=(s == n - 1)), r=[slab, U], w=[ps])
                cb(ps, kind, j)

        def smt():
            t = sm[self.sm_i % 6]
            self.sm_i += 1
            return t

        def filters(o, c0):
            for dr in range(2):
                col = dr * 2 * D + o * D + c0
                p.dma(w4t[:, dr, :], hy["hy_f_w4"][:, col:col + CT], r=[hy["hy_f_w4"]], w=[w4t])
            for seg in segs:
                off, n = seg[0], seg[1]
                si = 0 if seg[5] == "l" else 1
                rn = rns[si]
                psn = self.psx
                for j in range(n):
                    w_ = wn[j % 2]
                    p.op("act", lambda e: e.activation(out=w_[:], in_=rows["dl"][:], func=AF.Exp,
                                                       scale=negt[:, off + j:off + j + 1]), r=[rows["dl"], negt], w=[w_])
                    fts = []
                    for dr in range(2):
                        ps = self.next_psf()
                        p.op("pe", lambda e: e.matmul(ps[:, 0:CT], lhsT=h3T[:, (off + j) * 128:(off + j + 1) * 128],
                                                      rhs=w4t[:, dr, :], start=True, stop=True), r=[h3T, w4t], w=[ps])
                        f = smt()
                        p.op("dve", lambda e: e.scalar_tensor_tensor(out=f[:], in0=w_[:], scalar=cfg_shift, in1=ps[:, 0:CT],
                                                                     op0=ALU.add, op1=ALU.mult), r=[w_, ps], w=[f])
                        if dr == 1 and j == 0:
                            p.op("dve", lambda e: e.memset(f[0:1, :], 0.0), w=[f])
                        a_ = smt()
                        p.op("act", lambda e: e.activation(out=a_[:], in_=f[:], func=AF.Abs), r=[f], w=[a_])
                        first = (dr == 0 and j == 0)
                        lastm = (dr == 1 and j == n - 1)
                        p.op("pe", lambda e: e.matmul(psn[:, 0:CT], lhsT=self.ones_f[:], rhs=a_[:], start=first, stop=lastm),
                             r=[self.ones_f, a_], w=[psn])
                        fts.append(f)
                    p.op("pool", lambda e: e.tensor_tensor(out=gp[:, off + j, :], in0=fts[0][:], in1=fts[1][:], op=ALU.add),
                         r=[fts[0], fts[1]], w=[gp])
                    p.op("pool", lambda e: e.tensor_tensor(out=gm[:, off + j, :], in0=fts[0][:], in1=fts[1][:], op=ALU.subtract),
                         r=[fts[0], fts[1]], w=[gm])
                p.op("dve", lambda e: e.reciprocal(out=rn[:], in_=psn[:, 0:CT]), r=[psn], w=[rn])

                def cbA(ps, kind, j):
                    p.op("dve", lambda e: e.tensor_tensor(out=KA[:, off + j, :], in0=ps[:, 0:CT], in1=rn[:], op=ALU.mult), r=[ps, rn], w=[KA])

                def cbB(ps, kind, j):
                    p.op("dve", lambda e: e.tensor_tensor(out=KB[:, off + j, :], in0=ps[:, 0:CT], in1=rn[:], op=ALU.mult), r=[ps, rn], w=[KB])

                def cbN(ps, kind, j):
                    p.op("act", lambda e: e.copy(out=KD0[si][:], in_=KA[:, off, :]), r=[KA], w=[KD0[si]])
                    p.op("dve", lambda e: e.tensor_tensor(out=KD0[si][0:1, :], in0=ps[0:1, 0:CT], in1=rn[0:1, :], op=ALU.mult),
                         r=[ps, rn], w=[KD0[si]])
                fwd(gp, seg, [("re", j) for j in range(n)], cbA)
                fwd(gm, seg, [("im", j) for j in range(n)], cbB)
                fwd(gp, seg, [("im", 0)], cbN)
                p.op("dve", lambda e: e.memset(KB[0:1, off, :], 0.0), w=[KB])

        def spectrum_mul(U, seg):
            off, n, WF, WI, yoff, nm = seg
            si = 0 if nm == "l" else 1
            hold = {}

            def cb(ps, kind, j):
                u = smt()
                p.op("act", lambda e: e.copy(out=u[:], in_=ps[:, 0:CT]), r=[ps], w=[u])
                hold[kind] = u
                if kind == "im":
                    ure, uim = hold["re"], hold["im"]
                    t1, t2, t3, t4 = smt(), smt(), smt(), smt()
                    kd = KD0[si] if j == 0 else None
                    p.op("dve", lambda e: e.tensor_tensor(out=t1[:], in0=ure[:], in1=KA[:, off + j, :], op=ALU.mult), r=[ure, KA], w=[t1])
                    p.op("pool", lambda e: e.tensor_tensor(out=t2[:], in0=uim[:], in1=KB[:, off + j, :], op=ALU.mult), r=[uim, KB], w=[t2])
                    p.op("dve", lambda e: e.tensor_tensor(out=Y[:, yoff + j, :], in0=t1[:], in1=t2[:], op=ALU.subtract), r=[t1, t2], w=[Y])
                    p.op("pool", lambda e: e.tensor_tensor(out=t3[:], in0=ure[:], in1=KB[:, off + j, :], op=ALU.mult), r=[ure, KB], w=[t3])
                    if kd is not None:
                        p.op("dve", lambda e: e.tensor_tensor(out=t4[:], in0=uim[:], in1=kd[:], op=ALU.mult), r=[uim, kd], w=[t4])
                    else:
                        p.op("dve", lambda e: e.tensor_tensor(out=t4[:], in0=uim[:], in1=KA[:, off + j, :], op=ALU.mult), r=[uim, KA], w=[t4])
                    p.op("pool", lambda e: e.tensor_tensor(out=Y[:, yoff + n + j, :], in0=t3[:], in1=t4[:], op=ALU.add), r=[t3, t4], w=[Y])
            parts = []
            for j in range(n):
                parts += [("re", j), ("im", j)]
            fwd(U, seg, parts, cb)

        def inverse(seg, cb):
            off, n, WF, WI, yoff, nm = seg
            for tc in range(n):
                slab = wis[self.slab_i % 2]
                self.slab_i += 1
                p.dma(slab[:, 0:2 * n, :], WI[tc * 128:(tc + 1) * 128, :].rearrange("q (s i) -> q s i", i=128), r=[WI], w=[slab])
                ps = self.next_psf()
                for ic in range(2 * n):
                    p.op("pe", lambda e: e.matmul(ps[:, 0:CT], lhsT=slab[:, ic, :], rhs=Y[:, yoff + ic, :], start=(ic == 0),
                                                  stop=(ic == 2 * n - 1)), r=[slab, Y], w=[ps])
                cb(ps, off + tc)

        cfg_shift = 0.05
        for c0 in range(0, D, CT):
            p.dma(rows["dl"][:], hy["deltas"][c0:c0 + CT].partition_broadcast(128), r=[hy["deltas"]], w=[rows["dl"]])
            p.dma(rows["sk0"][:], hy["hy_skip"][0, c0:c0 + CT].partition_broadcast(128), r=[hy["hy_skip"]], w=[rows["sk0"]])
            p.dma(rows["sk1"][:], hy["hy_skip"][1, c0:c0 + CT].partition_broadcast(128), r=[hy["hy_skip"]], w=[rows["sk1"]])
            short_conv(0, c0, V)
            p.op("act", lambda e: e.copy(out=Ub[:], in_=V[:]), r=[V], w=[Ub])
            for o in range(2):
                filters(o, c0)
                for seg in segs:
                    spectrum_mul(Ub, seg)
                short_conv(1 + o, c0, X)
                sk = rows["sk%d" % o]

                def cb(ps, ch):
                    t1 = smt()
                    p.op("pool", lambda e: e.tensor_tensor(out=t1[:], in0=V[:, ch, :], in1=sk[:], op=ALU.mult), r=[V, sk], w=[t1])
                    p.op("dve", lambda e: e.tensor_tensor(out=t1[:], in0=t1[:], in1=ps[:, 0:CT], op=ALU.add), r=[t1, ps], w=[t1])
                    p.op("pool", lambda e: e.tensor_tensor(out=V[:, ch, :], in0=t1[:], in1=X[:, ch, :], op=ALU.mult), r=[t1, X], w=[V])
                for seg in segs:
                    inverse(seg, cb)
                p.op("act", lambda e: e.copy(out=Ub[:], in_=V[:]), r=[V], w=[Ub])
            for kk in range(CT // 128):
                for j0 in range(0, NCH, 8):
                    jn = min(8, NCH - j0)
                    pb = self.next_psb()
                    for j in range(j0, j0 + jn):
                        p.op("pe", lambda e: e.transpose(out=pb[:, (j - j0) * 128:(j - j0 + 1) * 128],
                                                         in_=Ub[:, j, kk * 128:(kk + 1) * 128], identity=self.ident_b[:]),
                             r=[Ub, self.ident_b], w=[pb])
                    p.op("act", lambda e: e.copy(out=zT_[:, kk, j0 * 128:(j0 + jn) * 128], in_=pb[:, 0:jn * 128]), r=[pb], w=[zT_])
                k = c0 // 128 + kk
                p.dma(ZTv[:, k, :, :], zT_[:, kk, :].rearrange("q (j t) -> q j t", t=128), r=[zT_], w=[self.ZT])
        p.barrier()


Builder.phase_hy_conv = phase_hy_conv


def phase_outproj(self, l, XTsrc, Wfull, bias_d, gate_chunk, tiles_x, tiles_c):
    cfg, p = self.cfg, self.p
    D, L = cfg["D"], cfg["L"]
    with ExitStack() as ph:
        self.modrows = p.sb([5, D], F32, "modrows", es=ph)
        gate = {}
        for stream, tiles in (("x", tiles_x), ("c", tiles_c)):
            if tiles:
                gate[stream] = p.sb([128, D], F32, "gate", es=ph)
                self.mod_tile(gate[stream], l, gate_chunk, stream)
        bias = None
        if bias_d is not None:
            bias = p.sb([128, D], F32, "obias", es=ph)
            self.bcast_row(bias, bias_d[:], bias_d, D)
        ht = [p.sb([128, 512], F32, "oh", es=ph) for _ in range(3)]
        tt = [p.sb([128, 512], F32, "ot", es=ph) for _ in range(3)]
        cnt = [0]

        def evac(ps, j, c0, c1):
            n = c1 - c0
            h_, t_ = ht[cnt[0] % 3], tt[cnt[0] % 3]
            cnt[0] += 1
            g = gate["x" if j < L // 128 else "c"]
            p.dma(h_[:, 0:n], self.HX[j * 128:(j + 1) * 128, c0:c1], r=[self.HX], w=[h_])
            if bias is not None:
                p.op("dve", lambda e: e.tensor_tensor(out=t_[:, 0:n], in0=ps[:, 0:n], in1=bias[:, c0:c1], op=ALU.add),
                     r=[ps, bias], w=[t_])
                p.op("pool", lambda e: e.tensor_tensor(out=t_[:, 0:n], in0=t_[:, 0:n], in1=g[:, c0:c1], op=ALU.mult),
                     r=[t_, g], w=[t_])
            else:
                p.op("dve", lambda e: e.tensor_tensor(out=t_[:, 0:n], in0=ps[:, 0:n], in1=g[:, c0:c1], op=ALU.mult),
                     r=[ps, g], w=[t_])
            p.op("dve", lambda e: e.tensor_tensor(out=h_[:, 0:n], in0=h_[:, 0:n], in1=t_[:, 0:n], op=ALU.add),
                 r=[h_, t_], w=[h_])
            p.dma(self.HX[j * 128:(j + 1) * 128, c0:c1], h_[:, 0:n], r=[h_], w=[self.HX])
        self.phase_proj(XTsrc, list(tiles_x) + list(tiles_c), Wfull, D, evac)


Builder.phase_outproj = phase_outproj


def hyena_consts(cfg):
    out = {}
    for nm, Lx in (("l", cfg["L"]), ("c", cfg["CTX"])):
        t = np.linspace(0.0, 1.0, Lx, dtype=np.float32)
        wpos = (np.float32(2.0 * math.pi / Lx) * np.arange(Lx, dtype=np.float32))
        nb = (cfg["EMB"] - 1) // 2
        bands = np.linspace(1e-4, nb - 1, nb, dtype=np.float32)
        z = np.concatenate([t[:, None], np.cos(bands[None, :] * wpos[:, None]), -np.sin(bands[None, :] * wpos[:, None])], axis=-1)
        out["zT_" + nm] = np.ascontiguousarray(z.T.astype(np.float32))
        out["negt_" + nm] = np.ascontiguousarray((-t).reshape(Lx // 128, 128).T.astype(np.float32))
    max_decay = math.log(1e-2) / 0.3
    min_decay = math.log(1e-2) / 1.5
    out["deltas"] = np.abs(np.linspace(min_decay, max_decay, cfg["D"], dtype=np.float32)).astype(np.float32)
    return out


def phase_peer(self, l, tiles_x, tiles_c):
    cfg, p = self.cfg, self.p
    D, L, KC = cfg["D"], cfg["L"], self.KC
    PH, PK, TOPK = cfg["PH"], cfg["PK"], cfg["TOPK"]
    E = PK * PK
    EC = E // 128
    EG = 8
    NG = EC // EG
    TG = 2
    NEG = -1.0e30
    if not hasattr(self, "pk"):
        self.pk = {}
        for ll in range(cfg["DEPTH"]):
            self.pk[ll] = (self.inp("peer_k1T%d" % ll, [128, PH, PK]), self.inp("peer_k2T%d" % ll, [128, PH, PK]))
    wq, uT, vv = self.W["peer_wq%d" % l], self.W["peer_uT%d" % l], self.W["peer_v%d" % l]
    vview = vv.ap().rearrange("(g c q) d -> g q c d", c=EG, q=128)
    tiles = list(tiles_x) + list(tiles_c)
    gate_d = {}
    with ExitStack() as ph0:
        self.modrows = p.sb([5, D], F32, "modrows", es=ph0)
        gtmp = p.sb([128, D], F32, "pgtmp", es=ph0)
        for stream, tl in (("x", tiles_x), ("c", tiles_c)):
            if tl:
                gate_d[stream] = p.dram([128, D], F32, "peer_gate_%d_%s" % (l, stream))
                self.mod_tile(gtmp, l, 5, stream)
                p.dma(gate_d[stream].ap(), gtmp[:], r=[gtmp], w=[gate_d[stream]])
        p.barrier()
    with ExitStack() as ph:
        sb = lambda shape, dt, name: p.sb(shape, dt, name, es=ph)
        gate1 = sb([128, D], F32, "pgate")
        gate_stream = [None]

        def set_gate(stream):
            if gate_stream[0] == stream:
                return
            gate_stream[0] = stream
            p.dma(gate1[:], gate_d[stream].ap(), r=[gate_d[stream]], w=[gate1])
        k1T = sb([128, PH, PK], F32, "k1T")
        k2T = sb([128, PH, PK], F32, "k2T")
        p.dma(k1T[:], self.pk[l][0].ap(), r=[self.pk[l][0]], w=[k1T])
        p.dma(k2T[:], self.pk[l][1].ap(), r=[self.pk[l][1]], w=[k2T])
        xg = sb([128, KC, TG * 128], BF16, "pxg")
        slab = [sb([128, KC, 128], BF16, "pslab") for _ in range(2)]
        qT = sb([128, 2 * PH, TG * 128], F32, "pqT")
        s1 = [sb([128, PH, PK], F32, "ps1") for _ in range(TG)]
        s2 = [sb([128, PH, PK], F32, "ps2") for _ in range(TG)]
        thr = [sb([128, PH], F32, "pthr") for _ in range(TG)]
        negb = [sb([128, PH], F32, "pnegb") for _ in range(TG)]
        acc = [sb([128, D], F32, "pacc") for _ in range(TG)]
        v1 = sb([128, TOPK], F32, "pv1")
        v2 = sb([128, TOPK], F32, "pv2")
        wk = sb([128, PK], F32, "pwk")
        cand = sb([128, TOPK * TOPK], F32, "pcand")
        cand2 = sb([128, TOPK * TOPK], F32, "pcand2")
        c16 = sb([128, TOPK], F32, "pc16")
        sc = [sb([128, 1], F32, "psc") for _ in range(4)]
        Tb = [sb([128, EG * 128], F32, "pT") for _ in range(2)]
        Eb = [sb([128, EG * 128], F32, "pE") for _ in range(2)]
        Cb = [sb([128, EG * 128], F32, "pC") for _ in range(2)]
        Gs = sb([128, EG * 128], F32, "pGs")
        Gb = sb([128, EG * 128], BF16, "pGb")
        gA = sb([128, EG, TG * 128], BF16, "pgA")
        CT = sb([128, EG, TG * 128], BF16, "pCT")
        Vt = [sb([128, EG, 512], BF16, "pVt") for _ in range(2)]
        hxs = [sb([128, 512], F32, "phx") for _ in range(2)]
        it = {"slab": 0, "blk": 0, "v": 0}

        def next_slab():
            s_ = slab[it["slab"] % 2]
            it["slab"] += 1
            return s_

        def top16(src_ap, src_buf, dst, n):
            work = wk if n == PK else cand2
            p.op("dve", lambda e: e.max(out=dst[:, 0:8], in_=src_ap), r=[src_buf], w=[dst])
            p.op("dve", lambda e: e.match_replace(out=work[:, 0:n], in_to_replace=dst[:, 0:8], in_values=src_ap, imm_value=NEG),
                 r=[src_buf, dst], w=[work])
            p.op("dve", lambda e: e.max(out=dst[:, 8:16], in_=work[:, 0:n]), r=[work], w=[dst])

        for g0 in range(0, len(tiles), TG):
            grp = tiles[g0:g0 + TG]
            ntok = len(grp) * 128
            for ti, j in enumerate(grp):
                p.dma(xg[:, :, ti * 128:(ti + 1) * 128], self.XT[j, :, :, :], r=[self.XT], w=[xg])
            for cc in range(2 * PH):
                s_ = next_slab()
                p.dma(s_[:], wq[cc * 128:(cc + 1) * 128, :].rearrange("q (k c) -> q k c", c=128), r=[wq], w=[s_])
                ps = self.next_psf()
                for k in range(KC):
                    p.op("pe", lambda e: e.matmul(ps[:, 0:ntok], lhsT=s_[:, k, :], rhs=xg[:, k, 0:ntok], start=(k == 0),
                                                  stop=(k == KC - 1)), r=[s_, xg], w=[ps])
                p.op("act", lambda e: e.copy(out=qT[:, cc, 0:ntok], in_=ps[:, 0:ntok]), r=[ps], w=[qT])
            for ti in range(len(grp)):
                for half, (kT, sdst) in enumerate(((k1T, s1[ti]), (k2T, s2[ti]))):
                    for h0 in range(0, PH, 4):
                        ps = self.next_psf()
                        for h in range(h0, h0 + 4):
                            p.op("pe", lambda e: e.matmul(ps[:, (h - h0) * PK:(h - h0 + 1) * PK],
                                                          lhsT=qT[:, 2 * h + half, ti * 128:(ti + 1) * 128], rhs=kT[:, h, :],
                                                          start=True, stop=True), r=[qT, kT], w=[ps])
                        p.op("act", lambda e: e.copy(out=sdst[:, h0:h0 + 4, :].rearrange("q h k -> q (h k)"), in_=ps[:, 0:4 * PK]),
                             r=[ps], w=[sdst])
                if "pk_notopk" in self.debug:
                    p.op("dve", lambda e: e.memset(thr[ti][:], 0.0), w=[thr[ti]])
                    p.op("dve", lambda e: e.memset(negb[ti][:], 0.0), w=[negb[ti]])
                for h in range(PH if "pk_notopk" not in self.debug else 0):
                    top16(s1[ti][:, h, :], s1[ti], v1, PK)
                    top16(s2[ti][:, h, :], s2[ti], v2, PK)
                    p.op("dve", lambda e: e.tensor_tensor(out=cand[:].rearrange("q (a b) -> q a b", b=TOPK),
                                                          in0=v1[:].unsqueeze(2).broadcast_to([128, TOPK, TOPK]),
                                                          in1=v2[:].unsqueeze(1).broadcast_to([128, TOPK, TOPK]), op=ALU.add),
                         r=[v1, v2], w=[cand])
                    top16(cand[:], cand, c16, TOPK * TOPK)
                    p.op("act", lambda e: e.copy(out=thr[ti][:, h:h + 1], in_=c16[:, 15:16]), r=[c16], w=[thr[ti]])
                    p.op("dve", lambda e: e.tensor_scalar(out=sc[0][:], in0=c16[:, 0:1], scalar1=-1.0, scalar2=None, op0=ALU.mult),
                         r=[c16], w=[sc[0]])
                    p.op("act", lambda e: e.activation(out=v1[:], in_=c16[:], func=AF.Exp, bias=sc[0][:, 0:1], accum_out=sc[1][:]),
                         r=[c16, sc[0]], w=[v1, sc[1]])
                    p.op("act", lambda e: e.activation(out=sc[2][:], in_=sc[1][:], func=AF.Ln), r=[sc[1]], w=[sc[2]])
                    p.op("dve", lambda e: e.tensor_tensor(out=negb[ti][:, h:h + 1], in0=sc[0][:], in1=sc[2][:], op=ALU.subtract),
                         r=[sc[0], sc[2]], w=[negb[ti]])
            for g in range(NG if "pk_noeg" not in self.debug else 0):
                for ci in range(EG):
                    ec = g * EG + ci
                    s_ = next_slab()
                    p.dma(s_[:], uT[ec * 128:(ec + 1) * 128, :].rearrange("q (k c) -> q k c", c=128), r=[uT], w=[s_])
                    ps = self.next_psf()
                    for k in range(KC):
                        p.op("pe", lambda e: e.matmul(ps[:, 0:ntok], lhsT=s_[:, k, :], rhs=xg[:, k, 0:ntok], start=(k == 0),
                                                      stop=(k == KC - 1)), r=[s_, xg], w=[ps])
                    p.op("act", lambda e: e.activation(out=gA[:, ci, 0:ntok], in_=ps[:, 0:ntok], func=AF.Gelu), r=[ps], w=[gA])
                for ti in range(len(grp)):
                    for h in range(PH):
                        b = it["blk"] % 2
                        it["blk"] += 1
                        T_, E_, C_ = Tb[b], Eb[b], Cb[b]
                        p.op("dve", lambda e: e.tensor_tensor(
                            out=T_[:].rearrange("q (a b) -> q a b", b=PK),
                            in0=s1[ti][:, h, g * EG:(g + 1) * EG].unsqueeze(2).broadcast_to([128, EG, PK]),
                            in1=s2[ti][:, h, :].unsqueeze(1).broadcast_to([128, EG, PK]), op=ALU.add),
                            r=[s1[ti], s2[ti]], w=[T_])
                        p.op("act", lambda e: e.activation(out=E_[:], in_=T_[:], func=AF.Exp, bias=negb[ti][:, h:h + 1]),
                             r=[T_, negb[ti]], w=[E_])
                        dst = Gs if h == 0 else C_
                        p.op("dve", lambda e: e.scalar_tensor_tensor(out=dst[:], in0=T_[:], scalar=thr[ti][:, h:h + 1], in1=E_[:],
                                                                     op0=ALU.is_ge, op1=ALU.mult), r=[T_, thr[ti], E_], w=[dst])
                        if h > 0:
                            p.op("pool", lambda e: e.tensor_tensor(out=Gs[:], in0=Gs[:], in1=C_[:], op=ALU.add), r=[Gs, C_], w=[Gs])
                    p.op("act", lambda e: e.copy(out=Gb[:], in_=Gs[:]), r=[Gs], w=[Gb])
                    pb = self.next_psb()
                    for ci in range(EG):
                        p.op("pe", lambda e: e.transpose(out=pb[:, ci * 128:(ci + 1) * 128], in_=Gb[:, ci * 128:(ci + 1) * 128],
                                                         identity=self.ident_b[:]), r=[Gb, self.ident_b], w=[pb])
                    p.op("dve", lambda e: e.tensor_tensor(out=CT[:, :, ti * 128:(ti + 1) * 128],
                                                          in0=pb[:, 0:EG * 128].rearrange("q (c t) -> q c t", t=128),
                                                          in1=gA[:, :, ti * 128:(ti + 1) * 128], op=ALU.mult), r=[pb, gA], w=[CT])
                for dt in range(0, D, 512):
                    dn = min(512, D - dt)
                    V_ = Vt[it["v"] % 2]
                    it["v"] += 1
                    p.dma(V_[:, :, 0:dn], vview[g, :, :, dt:dt + dn], r=[vv], w=[V_])
                    for ti in range(len(grp)):
                        ps = self.next_psf()
                        for ci in range(EG):
                            p.op("pe", lambda e: e.matmul(ps[:, 0:dn], lhsT=CT[:, ci, ti * 128:(ti + 1) * 128], rhs=V_[:, ci, 0:dn],
                                                          start=(ci == 0), stop=(ci == EG - 1)), r=[CT, V_], w=[ps])
                        if g == 0:
                            p.op("act", lambda e: e.copy(out=acc[ti][:, dt:dt + dn], in_=ps[:, 0:dn]), r=[ps], w=[acc[ti]])
                        else:
                            p.op("dve", lambda e: e.tensor_tensor(out=acc[ti][:, dt:dt + dn], in0=acc[ti][:, dt:dt + dn],
                                                                  in1=ps[:, 0:dn], op=ALU.add), r=[acc[ti], ps], w=[acc[ti]])
            for ti, j in enumerate(grp):
                if "pk_noeg" in self.debug:
                    p.op("dve", lambda e: e.memset(acc[ti][:], 0.0), w=[acc[ti]])
                set_gate("x" if j < L // 128 else "c")
                gt_ = gate1
                p.op("pool", lambda e: e.tensor_tensor(out=acc[ti][:], in0=acc[ti][:], in1=gt_[:], op=ALU.mult), r=[acc[ti], gt_], w=[acc[ti]])
                for ci_, c0 in enumerate(range(0, D, 512)):
                    hx = hxs[ci_ % 2]
                    p.dma(hx[:], self.HX[j * 128:(j + 1) * 128, c0:c0 + 512], r=[self.HX], w=[hx])
                    p.op("dve", lambda e: e.tensor_tensor(out=hx[:], in0=hx[:], in1=acc[ti][:, c0:c0 + 512], op=ALU.add), r=[hx, acc[ti]], w=[hx])
                    p.dma(self.HX[j * 128:(j + 1) * 128, c0:c0 + 512], hx[:], r=[hx], w=[self.HX])
        p.barrier()


Builder.phase_peer = phase_peer


def phase_na_qkv(self, l):
    cfg, p = self.cfg, self.p
    D, L, CTX, T, HD = cfg["D"], cfg["L"], cfg["CTX"], self.T, cfg["HD"]
    if not hasattr(self, "QK"):
        self.QK = p.dram([T, 2 * D], BF16, "QK")
        self.VV = p.dram([T, D], BF16, "VV")
        self.OO = p.dram([L, D], BF16, "OO")
        self.na = {"q_g": self.inp("na_q_g", [HD]), "k_g": self.inp("na_k_g", [HD]),
                   "rpbx": self.inp("na_rpbx", [D // HD, 128, 14, cfg["GW"]]), "maskT": self.inp("na_maskT", [128, cfg["GW"]])}
    with ExitStack() as ph:
        gq = p.sb([128, HD], F32, "gq", es=ph)
        gk = p.sb([128, HD], F32, "gk", es=ph)
        self.bcast_row(gq, self.na["q_g"][:], self.na["q_g"], HD)
        self.bcast_row(gk, self.na["k_g"][:], self.na["k_g"], HD)
        p.op("dve", lambda e: e.tensor_scalar(out=gq[:], in0=gq[:], scalar1=float(HD) ** -0.5, scalar2=None, op0=ALU.mult),
             r=[gq], w=[gq])
        sq = [p.sb([128, 512], F32, "qsq", es=ph) for _ in range(2)]
        xs = [p.sb([128, 512], F32, "qxs", es=ph) for _ in range(2)]
        ss = [p.sb([128, 4], F32, "qss", es=ph) for _ in range(2)]
        ob = [p.sb([128, 512], BF16, "qob", es=ph) for _ in range(3)]
        cnt = [0]
        NH = 512 // HD

        def evac(ps, j, c0, c1):
            i = cnt[0]
            cnt[0] += 1
            o = ob[i % 3]
            kind = c0 // D
            if kind == 2:
                p.op("act", lambda e: e.copy(out=o[:], in_=ps[:, 0:512]), r=[ps], w=[o])
                p.dma(self.VV[j * 128:(j + 1) * 128, c0 - 2 * D:c1 - 2 * D], o[:], r=[o], w=[self.VV])
                return
            if kind == 0 and j >= L // 128:
                return
            s_, x_, r_ = sq[i % 2], xs[i % 2], ss[i % 2]
            g = gq if kind == 0 else gk
            p.op("act", lambda e: e.activation(out=s_[:], in_=ps[:, 0:512], func=AF.Square), r=[ps], w=[s_])
            p.op("dve", lambda e: e.tensor_reduce(out=r_[:], in_=s_[:].rearrange("q (h d) -> q h d", d=HD), axis=AX.X, op=ALU.add),
                 r=[s_], w=[r_])
            p.op("dve", lambda e: e.tensor_scalar(out=r_[:], in0=r_[:], scalar1=1.0 / HD, scalar2=1e-6, op0=ALU.mult, op1=ALU.add),
                 r=[r_], w=[r_])
            p.op("act", lambda e: e.activation(out=r_[:], in_=r_[:], func=AF.Sqrt), r=[r_], w=[r_])
            p.op("dve", lambda e: e.reciprocal(out=r_[:], in_=r_[:]), r=[r_], w=[r_])
            p.op("dve", lambda e: e.tensor_tensor(out=x_[:].rearrange("q (h d) -> q h d", d=HD),
                                                  in0=ps[:, 0:512].rearrange("q (h d) -> q h d", d=HD),
                                                  in1=r_[:].unsqueeze(2).broadcast_to([128, NH, HD]), op=ALU.mult), r=[ps, r_], w=[x_])
            p.op("pool", lambda e: e.tensor_tensor(out=o[:].rearrange("q (h d) -> q h d", d=HD),
                                                   in0=x_[:].rearrange("q (h d) -> q h d", d=HD),
                                                   in1=g[:].unsqueeze(1).broadcast_to([128, NH, HD]), op=ALU.mult), r=[x_, g], w=[o])
            p.dma(self.QK[j * 128:(j + 1) * 128, c0:c1], o[:], r=[o], w=[self.QK])
        self.phase_proj(self.XT, list(range(T // 128)), self.W["na_w_qkv"], 3 * D, evac)


Builder.phase_na_qkv = phase_na_qkv


def phase_na_attn(self, l):
    cfg, p = self.cfg, self.p
    D, L, CTX, T, HD, GW = cfg["D"], cfg["L"], cfg["CTX"], self.T, cfg["HD"], cfg["GW"]
    H = D // HD
    R = L // GW
    KH = min(cfg["WIN_H"], R)
    assert KH == 8 and GW == 64 and HD == 128
    NTQ, NTK, NCX = L // 128, T // 128, CTX // 128
    QK, VV, OO = self.QK, self.VV, self.OO
    with ExitStack() as ph:
        sb = lambda shape, dt, name: p.sb(shape, dt, name, es=ph)
        maskT = sb([128, GW], F32, "maskT")
        p.dma(maskT[:], self.na["maskT"][:, :], r=[self.na["maskT"]], w=[maskT])
        qtok = sb([128, NTQ, HD], BF16, "aqtok")
        ktok = sb([128, NTK, HD], BF16, "aktok")
        qT = sb([128, L], BF16, "aqT")
        kT = sb([128, T], BF16, "akT")
        Va = sb([128, NTK, HD + 1], BF16, "aVa")
        Vs = sb([128, NTQ - 1, HD + 1], BF16, "aVs")
        p.op("dve", lambda e: e.memset(Va[:], 1.0), w=[Va])
        p.op("dve", lambda e: e.memset(Vs[:], 1.0), w=[Vs])
        bias = sb([128, 14, GW], F32, "abias")
        tl = [sb([128, 4 * GW], F32, "atl") for _ in range(2)]
        PT = [sb([128, 6, GW], BF16, "aPT") for _ in range(2)]
        rz = [sb([GW, 1], F32, "arz") for _ in range(2)]
        oe = [sb([GW, R // 2, HD], BF16, "aoe") for _ in range(2)]
        it = 0
        for h in range(H):
            p.dma(qtok[:], QK[0:L, h * HD:(h + 1) * HD].rearrange("(j q) d -> q j d", q=128), r=[QK], w=[qtok])
            p.dma(ktok[:], QK[:, D + h * HD:D + (h + 1) * HD].rearrange("(j q) d -> q j d", q=128), r=[QK], w=[ktok])
            p.dma(Va[:, :, 0:HD], VV[:, h * HD:(h + 1) * HD].rearrange("(j q) d -> q j d", q=128), r=[VV], w=[Va])
            p.dma(Vs[:, :, 0:HD], VV[GW:L - GW, h * HD:(h + 1) * HD].rearrange("(j q) d -> q j d", q=128), r=[VV], w=[Vs])
            p.dma(bias[:], self.na["rpbx"][h, :, :, :], r=[self.na["rpbx"]], w=[bias])
            p.op("dve", lambda e: e.tensor_tensor(out=bias[:], in0=bias[:], in1=maskT[:].unsqueeze(1).broadcast_to([128, 14, GW]),
                                                  op=ALU.add), r=[bias, maskT], w=[bias])
            for src, dst, n in ((qtok, qT, NTQ), (ktok, kT, NTK)):
                for j0 in range(0, n, 8):
                    jn = min(8, n - j0)
                    pb = self.next_psb()
                    for j in range(j0, j0 + jn):
                        p.op("pe", lambda e: e.transpose(out=pb[:, (j - j0) * 128:(j - j0 + 1) * 128], in_=src[:, j, :],
                                                         identity=self.ident_b[:]), r=[src, self.ident_b], w=[pb])
                    p.op("act", lambda e: e.copy(out=dst[:, j0 * 128:(j0 + jn) * 128], in_=pb[:, 0:jn * 128]), r=[pb], w=[dst])
            for r in range(R):
                r0 = min(max(r - KH // 2, 0), R - KH)
                a0 = 7 - (r - r0)
                i = it % 2
                it += 1
                ps = self.next_psf()
                for kc in range(4):
                    t0 = (r0 + 2 * kc) * GW
                    p.op("pe", lambda e: e.matmul(ps[:, kc * GW:(kc + 1) * GW], lhsT=kT[:, t0:t0 + 128], rhs=qT[:, r * GW:(r + 1) * GW],
                                                  start=True, stop=True), r=[kT, qT], w=[ps])
                for cx in range(NCX):
                    p.op("pe", lambda e: e.matmul(ps[:, (4 + cx) * GW:(5 + cx) * GW], lhsT=kT[:, L + cx * 128:L + (cx + 1) * 128],
                                                  rhs=qT[:, r * GW:(r + 1) * GW], start=True, stop=True), r=[kT, qT], w=[ps])
                t_, P_ = tl[i], PT[i]
                p.op("dve", lambda e: e.tensor_tensor(out=t_[:].rearrange("q (c k) -> q c k", k=GW),
                                                      in0=ps[:, 0:4 * GW].rearrange("q (c k) -> q c k", k=GW),
                                                      in1=bias[:, a0:a0 + 7:2, :], op=ALU.add), r=[ps, bias], w=[t_])
                p.op("act", lambda e: e.activation(out=P_[:, 0:4, :].rearrange("q c k -> q (c k)"), in_=t_[:], func=AF.Exp),
                     r=[t_], w=[P_])
                p.op("act", lambda e: e.activation(out=P_[:, 4:4 + NCX, :].rearrange("q c k -> q (c k)"),
                                                   in_=ps[:, 4 * GW:(4 + NCX) * GW], func=AF.Exp), r=[ps], w=[P_])
                po = self.next_psf()
                nmm = 4 + NCX
                for kc in range(4):
                    if r0 % 2 == 0:
                        V_ = Va[:, r0 // 2 + kc, :]
                    else:
                        V_ = Vs[:, (r0 - 1) // 2 + kc, :]
                    p.op("pe", lambda e: e.matmul(po[0:GW, 0:HD + 1], lhsT=P_[:, kc, :], rhs=V_, start=(kc == 0), stop=False),
                         r=[P_, Va, Vs], w=[po])
                for cx in range(NCX):
                    p.op("pe", lambda e: e.matmul(po[0:GW, 0:HD + 1], lhsT=P_[:, 4 + cx, :], rhs=Va[:, NTQ + cx, :], start=False,
                                                  stop=(cx == NCX - 1)), r=[P_, Va], w=[po])
                z_ = rz[i]
                p.op("dve", lambda e: e.reciprocal(out=z_[:], in_=po[0:GW, HD:HD + 1]), r=[po], w=[z_])
                o_ = oe[r % 2]
                p.op("act", lambda e: e.activation(out=o_[:, r // 2, :], in_=po[0:GW, 0:HD], func=AF.Copy, scale=z_[:, 0:1]),
                     r=[po, z_], w=[o_])
            for par in range(2):
                p.dma(OO[:, h * HD:(h + 1) * HD].rearrange("(i two q) d -> two q i d", two=2, q=GW)[par], oe[par][:],
                      r=[oe[par]], w=[OO])
        p.barrier()


Builder.phase_na_attn = phase_na_attn


def phase_transpose(self, SRC, tiles, XT):
    cfg, p = self.cfg, self.p
    D, KC = cfg["D"], self.KC
    with ExitStack() as ph:
        yb = [p.sb([128, D], BF16, "ty", es=ph) for _ in range(2)]
        xT = [p.sb([128, KC, 128], BF16, "txT", es=ph) for _ in range(2)]
        for it, j in enumerate(tiles):
            y_, xT_ = yb[it % 2], xT[it % 2]
            p.dma(y_[:], SRC[j * 128:(j + 1) * 128, :], r=[SRC], w=[y_])
            for k0 in range(0, KC, 8):
                pb = self.next_psb()
                kn = min(8, KC - k0)
                for k in range(k0, k0 + kn):
                    p.op("pe", lambda e: e.transpose(out=pb[:, (k - k0) * 128:(k - k0 + 1) * 128], in_=y_[:, k * 128:(k + 1) * 128],
                                                     identity=self.ident_b[:]), r=[y_, self.ident_b], w=[pb])
                p.op("act", lambda e: e.copy(out=xT_[:, k0:k0 + kn, :].rearrange("q k t -> q (k t)"), in_=pb[:, 0:kn * 128]),
                     r=[pb], w=[xT_])
            p.dma(XT[j, :, :, :], xT_[:], r=[xT_], w=[XT])
        p.barrier()


Builder.phase_transpose = phase_transpose


def na_consts(cfg, rpb):
    GW, WW = cfg["GW"], cfg["WIN_W"]
    cols = np.arange(GW)
    c_start = np.clip(cols - WW // 2, 0, GW - WW)
    col_in = (cols[None, :] >= c_start[:, None]) & (cols[None, :] < c_start[:, None] + WW)
    col_idx = np.clip(cols[None, :] - cols[:, None], -(WW - 1), WW - 1) + (WW - 1)
    H = rpb.shape[0]
    rc = rpb[:, :, col_idx]
    rcT = rc.transpose(0, 1, 3, 2)
    out = np.zeros((H, 128, 14, GW), np.float32)
    for a in range(14):
        out[:, 0:GW, a, :] = rcT[:, a]
        out[:, GW:2 * GW, a, :] = rcT[:, a + 1]
    m = np.where(col_in.T, 0.0, -30000.0).astype(np.float32)
    maskT = np.concatenate([m, m], axis=0)
    return out, maskT
```
